# Optimizing a Trainium2 kernel written in Bass

```python
import math
import jax, jax.numpy as jnp
from jax import lax
import numpy as np

D_MODEL = 1024
BATCH = 16
SEQ = 2048
DEPTH = 2

NSA_DH = 64
NSA_HEADS = (D_MODEL // 2) // NSA_DH
NSA_KV_GROUPS = 2
NSA_HPG = NSA_HEADS // NSA_KV_GROUPS
NSA_WIDTH = NSA_HEADS * NSA_DH
NSA_KV_WIDTH = NSA_KV_GROUPS * NSA_DH
CMP_LEN = 32
CMP_STRIDE = 16
CMP_HIDDEN = 2 * NSA_DH
SLC_BLOCK = 64
N_SELECT = 16
WINDOW = 512
Q_BLOCK = 64
ROPE_THETA = 500000.0
ROPE_DIMS = NSA_DH // 4
MLSTM_HEADS = 4
MLSTM_DH = (D_MODEL - NSA_WIDTH) // MLSTM_HEADS
MLSTM_WIDTH = MLSTM_HEADS * MLSTM_DH
MLSTM_CHUNK = 64
MLSTM_CONV = 4
D_MIX = NSA_WIDTH + MLSTM_WIDTH
D_FF = ((8 * D_MODEL // 3 + 127) // 128) * 128
EPS = 1e-6
NEG_INF = -1e30
FORCE_SCORE = 1e9
IN_SIZES = [NSA_WIDTH] + [NSA_KV_WIDTH] * 6 + [3 * NSA_HEADS] + [MLSTM_WIDTH] * 4 + [MLSTM_HEADS] * 2
IN_COLS = sum(IN_SIZES)
IN_SPLITS = [int(s) for s in np.cumsum(IN_SIZES)[:-1]]

kernel_name = 'hybrid_nsa_mlstm_macaron_sandwich'


def rms_norm(x, g):
    x32 = x.astype(jnp.float32)
    y = x32 * lax.rsqrt(jnp.mean(x32 * x32, axis=-1, keepdims=True) + EPS)
    return (y * g.astype(jnp.float32)).astype(x.dtype)


def swiglu(x, w_gu, w_down):
    gate, up = jnp.split(x @ w_gu, 2, axis=-1)
    return (jax.nn.silu(gate) * up) @ w_down


def rope_tables(seq):
    pos = jnp.arange(seq, dtype=jnp.float32)
    inv_freq = ROPE_THETA ** (-jnp.arange(0, ROPE_DIMS, 2, dtype=jnp.float32) / ROPE_DIMS)
    ang = pos[:, None] * inv_freq[None, :]
    return jnp.cos(ang), jnp.sin(ang)


def partial_rope(x, cos, sin):
    xr, xp = x[..., :ROPE_DIMS], x[..., ROPE_DIMS:]
    x1, x2 = jnp.split(xr, 2, axis=-1)
    c, s = cos[None, :, None, :], sin[None, :, None, :]
    rot = jnp.concatenate([x1 * c - x2 * s, x2 * c + x1 * s], axis=-1).astype(x.dtype)
    return jnp.concatenate([rot, xp], axis=-1)


def compress_blocks(tok, pe, w1, w2):
    b, s = tok.shape[:2]
    n_cmp = (s - CMP_LEN) // CMP_STRIDE + 1
    idx = np.arange(n_cmp)[:, None] * CMP_STRIDE + np.arange(CMP_LEN)[None, :]
    blocks = tok[:, idx] + pe[None, None, :, None, :].astype(tok.dtype)
    flat = blocks.transpose(0, 3, 1, 2, 4).reshape(b, NSA_KV_GROUPS, n_cmp, CMP_LEN * NSA_DH)
    return jax.nn.gelu(flat @ w1) @ w2


def nsa_group(q, k_cmp, v_cmp, k_slc, v_slc, k_win, v_win, gates, pe, w1, w2, cos, sin):
    b, s = q.shape[:2]
    dt = q.dtype
    q = partial_rope(q.reshape(b, s, NSA_HEADS, NSA_DH), cos, sin)
    q = q.reshape(b, s, NSA_KV_GROUPS, NSA_HPG, NSA_DH)
    kv_shape = (b, s, NSA_KV_GROUPS, NSA_DH)
    k_slc = partial_rope(k_slc.reshape(kv_shape), cos, sin)
    k_win = partial_rope(k_win.reshape(kv_shape), cos, sin)
    v_slc = v_slc.reshape(kv_shape)
    v_win = v_win.reshape(kv_shape)
    kc = compress_blocks(k_cmp.reshape(kv_shape), pe[0], w1[0], w2[0])
    vc = compress_blocks(v_cmp.reshape(kv_shape), pe[1], w1[1], w2[1])
    n_cmp = kc.shape[2]
    cmp_end = jnp.arange(n_cmp) * CMP_STRIDE + CMP_LEN - 1
    n_slc = s // SLC_BLOCK
    n_sel = min(N_SELECT, n_slc)
    ci = np.arange(n_cmp)[:, None] * CMP_STRIDE
    sj = np.arange(n_slc)[None, :] * SLC_BLOCK
    overlap = jnp.asarray(((ci < sj + SLC_BLOCK) & (ci + CMP_LEN > sj)).astype(np.float32))
    ks_blk = k_slc.reshape(b, n_slc, SLC_BLOCK, NSA_KV_GROUPS, NSA_DH).transpose(0, 3, 1, 2, 4)
    vs_blk = v_slc.reshape(b, n_slc, SLC_BLOCK, NSA_KV_GROUPS, NSA_DH).transpose(0, 3, 1, 2, 4)
    kw_pad = jnp.pad(k_win, ((0, 0), (WINDOW, 0), (0, 0), (0, 0)))
    vw_pad = jnp.pad(v_win, ((0, 0), (WINDOW, 0), (0, 0), (0, 0)))
    scale = NSA_DH ** -0.5
    n_qb = s // Q_BLOCK
    q_blocks = jnp.moveaxis(q.reshape(b, n_qb, Q_BLOCK, NSA_KV_GROUPS, NSA_HPG, NSA_DH), 1, 0)
    b_ix = jnp.arange(b)[:, None, None, None]
    g_ix = jnp.arange(NSA_KV_GROUPS)[None, :, None, None]
    blk = jnp.arange(n_slc)
    r_off = jnp.arange(SLC_BLOCK)

    def block_fn(args):
        c, qb = args
        t = c * Q_BLOCK + jnp.arange(Q_BLOCK)
        s_c = jnp.einsum('bqghd,bgnd->bghqn', qb, kc).astype(jnp.float32) * scale
        valid_c = cmp_end[None, :] <= t[:, None]
        p_c = jax.nn.softmax(jnp.where(valid_c, s_c, NEG_INF), axis=-1)
        p_c = p_c * jnp.any(valid_c, axis=-1)[:, None].astype(jnp.float32)
        o_c = jnp.einsum('bghqn,bgnd->bqghd', p_c.astype(dt), vc)
        imp = jnp.einsum('bghqn,nj->bgqj', p_c, overlap)
        cur = t // SLC_BLOCK
        forced = (blk[None, :] == 0) | (blk[None, :] == cur[:, None]) | (blk[None, :] == cur[:, None] - 1)
        blk_valid = blk[None, :] * SLC_BLOCK <= t[:, None]
        imp = jnp.where(forced, FORCE_SCORE, imp)
        imp = jnp.where(blk_valid, imp, NEG_INF)
        _, idx = lax.top_k(imp, n_sel)
        k_sel = ks_blk[b_ix, g_ix, idx]
        v_sel = vs_blk[b_ix, g_ix, idx]
        tok_pos = idx[..., None] * SLC_BLOCK + r_off
        valid_s = tok_pos <= t[None, None, :, None, None]
        s_s = jnp.einsum('bqghd,bgqnrd->bghqnr', qb, k_sel).astype(jnp.float32) * scale
        s_s = jnp.where(valid_s[:, :, None], s_s, NEG_INF)
        p_s = jax.nn.softmax(s_s.reshape(s_s.shape[:4] + (-1,)), axis=-1).reshape(s_s.shape)
        o_s = jnp.einsum('bghqnr,bgqnrd->bqghd', p_s.astype(dt), v_sel)
        start = c * Q_BLOCK
        k_w = lax.dynamic_slice_in_dim(kw_pad, start, Q_BLOCK + WINDOW, axis=1)
        v_w = lax.dynamic_slice_in_dim(vw_pad, start, Q_BLOCK + WINDOW, axis=1)
        kpos = start - WINDOW + jnp.arange(Q_BLOCK + WINDOW)
        valid_w = (kpos[None, :] <= t[:, None]) & (kpos[None, :] > t[:, None] - WINDOW) & (kpos[None, :] >= 0)
        s_w = jnp.einsum('bqghd,bkgd->bghqk', qb, k_w).astype(jnp.float32) * scale
        p_w = jax.nn.softmax(jnp.where(valid_w, s_w, NEG_INF), axis=-1)
        o_w = jnp.einsum('bghqk,bkgd->bqghd', p_w.astype(dt), v_w)
        return o_c, o_s, o_w

    o_c, o_s, o_w = lax.map(block_fn, (jnp.arange(n_qb), q_blocks))
    unblock = lambda o: jnp.moveaxis(o, 0, 1).reshape(b, s, NSA_HEADS, NSA_DH).astype(jnp.float32)
    g = jax.nn.sigmoid(gates.astype(jnp.float32)).reshape(b, s, 3, NSA_HEADS, 1)
    out = g[:, :, 0] * unblock(o_c) + g[:, :, 1] * unblock(o_s) + g[:, :, 2] * unblock(o_w)
    return out.reshape(b, s, NSA_WIDTH).astype(dt)


def mlstm_group(q, k, v, o_pre, i_pre, f_pre, conv_w, conv_b, norm_g, i_bias, f_bias):
    b, s = q.shape[:2]
    dt = q.dtype
    qk = jnp.concatenate([q, k], axis=-1)
    ch = qk.shape[-1]
    qk = lax.conv_general_dilated(qk, conv_w.astype(qk.dtype)[:, None, :], window_strides=(1,),
                                  padding=[(MLSTM_CONV - 1, 0)], dimension_numbers=('NWC', 'WIO', 'NWC'),
                                  feature_group_count=ch) + conv_b.astype(qk.dtype)
    q, k = jnp.split(jax.nn.silu(qk), 2, axis=-1)
    heads = lambda a: a.reshape(b, s, MLSTM_HEADS, MLSTM_DH).transpose(0, 2, 1, 3).astype(jnp.float32)
    q, k, v = heads(q), heads(k) * (MLSTM_DH ** -0.5), heads(v)
    i_log = (i_pre.astype(jnp.float32) + i_bias.astype(jnp.float32)).transpose(0, 2, 1)
    f_log = jax.nn.log_sigmoid(f_pre.astype(jnp.float32) + f_bias.astype(jnp.float32)).transpose(0, 2, 1)
    L = MLSTM_CHUNK
    nc = s // L
    chunk = lambda a: jnp.moveaxis(a.reshape(a.shape[:2] + (nc, L) + a.shape[3:]), 2, 0)
    xs = (chunk(q), chunk(k), chunk(v), chunk(i_log), chunk(f_log))
    tril = jnp.tril(jnp.ones((L, L), dtype=bool))

    def step(carry, inp):
        C, n, m = carry
        qc, kc, vc, ic, fc = inp
        bcum = jnp.cumsum(fc, axis=-1)
        D = jnp.where(tril, bcum[..., :, None] - bcum[..., None, :] + ic[..., None, :], NEG_INF)
        inter = bcum + m[..., None]
        m_t = jnp.maximum(jnp.max(D, axis=-1), inter)
        w_in = jnp.exp(D - m_t[..., None])
        w_prev = jnp.exp(inter - m_t)
        sc = jnp.einsum('bhtd,bhsd->bhts', qc, kc) * w_in
        num = jnp.einsum('bhts,bhsd->bhtd', sc, vc) + w_prev[..., None] * jnp.einsum('bhtd,bhde->bhte', qc, C)
        den = jnp.sum(sc, axis=-1) + w_prev * jnp.einsum('bhtd,bhd->bht', qc, n)
        h = num / jnp.maximum(jnp.abs(den), jnp.exp(-m_t))[..., None]
        b_last = bcum[..., -1]
        dec = b_last[..., None] - bcum + ic
        m_new = jnp.maximum(b_last + m, jnp.max(dec, axis=-1))
        wk = jnp.exp(dec - m_new[..., None])
        carry_scale = jnp.exp(b_last + m - m_new)
        C_new = carry_scale[..., None, None] * C + jnp.einsum('bhs,bhsd,bhse->bhde', wk, kc, vc)
        n_new = carry_scale[..., None] * n + jnp.einsum('bhs,bhsd->bhd', wk, kc)
        return (C_new, n_new, m_new), h

    init = (jnp.zeros((b, MLSTM_HEADS, MLSTM_DH, MLSTM_DH), jnp.float32),
            jnp.zeros((b, MLSTM_HEADS, MLSTM_DH), jnp.float32),
            jnp.zeros((b, MLSTM_HEADS), jnp.float32))
    _, hs = lax.scan(step, init, xs)
    h = jnp.moveaxis(hs, 0, 2).reshape(b, MLSTM_HEADS, s, MLSTM_DH).transpose(0, 2, 1, 3)
    h = h * lax.rsqrt(jnp.mean(h * h, axis=-1, keepdims=True) + EPS)
    h = h * norm_g.astype(jnp.float32).reshape(MLSTM_HEADS, MLSTM_DH)
    o = jax.nn.sigmoid(o_pre.astype(jnp.float32)).reshape(b, s, MLSTM_HEADS, MLSTM_DH)
    return (h * o).reshape(b, s, MLSTM_WIDTH).astype(dt)


def setup_inputs(seed: int = 0) -> dict:
    key = jax.random.key(seed)
    ks = jax.random.split(key, 16)
    nrm = lambda k, shape, sc: jax.random.normal(k, shape, jnp.float32) * sc
    return {
        'x': nrm(ks[0], (BATCH, SEQ, D_MODEL), 1.0),
        'norm_g': 1.0 + nrm(ks[1], (DEPTH, 6, D_MODEL), 0.02),
        'ffn1_w_gu': nrm(ks[2], (DEPTH, D_MODEL, 2 * D_FF), D_MODEL ** -0.5),
        'ffn1_w_down': nrm(ks[3], (DEPTH, D_FF, D_MODEL), D_FF ** -0.5),
        'ffn2_w_gu': nrm(ks[4], (DEPTH, D_MODEL, 2 * D_FF), D_MODEL ** -0.5),
        'ffn2_w_down': nrm(ks[5], (DEPTH, D_FF, D_MODEL), D_FF ** -0.5),
        'mix_w_in': nrm(ks[6], (DEPTH, D_MODEL, IN_COLS), D_MODEL ** -0.5),
        'mix_w_out': nrm(ks[7], (DEPTH, D_MIX, D_MODEL), D_MIX ** -0.5),
        'nsa_cmp_pe': nrm(ks[8], (DEPTH, 2, CMP_LEN, NSA_DH), 0.02),
        'nsa_cmp_w1': nrm(ks[9], (DEPTH, 2, CMP_LEN * NSA_DH, CMP_HIDDEN), (CMP_LEN * NSA_DH) ** -0.5),
        'nsa_cmp_w2': nrm(ks[10], (DEPTH, 2, CMP_HIDDEN, NSA_DH), CMP_HIDDEN ** -0.5),
        'mlstm_conv_w': nrm(ks[11], (DEPTH, MLSTM_CONV, 2 * MLSTM_WIDTH), MLSTM_CONV ** -0.5),
        'mlstm_conv_b': nrm(ks[12], (DEPTH, 2 * MLSTM_WIDTH), 0.02),
        'mlstm_i_bias': nrm(ks[13], (DEPTH, MLSTM_HEADS), 0.1),
        'mlstm_f_bias': jnp.linspace(3.0, 6.0, MLSTM_HEADS, dtype=jnp.float32)[None, :] + nrm(ks[14], (DEPTH, MLSTM_HEADS), 0.1),
        'mlstm_norm_g': 1.0 + nrm(ks[15], (DEPTH, MLSTM_WIDTH), 0.02),
    }


def reference(x, norm_g, ffn1_w_gu, ffn1_w_down, ffn2_w_gu, ffn2_w_down, mix_w_in, mix_w_out,
              nsa_cmp_pe, nsa_cmp_w1, nsa_cmp_w2, mlstm_conv_w, mlstm_conv_b, mlstm_i_bias,
              mlstm_f_bias, mlstm_norm_g):
    cos, sin = rope_tables(x.shape[1])
    for l in range(DEPTH):
        g = norm_g[l]
        h = swiglu(rms_norm(x, g[0]), ffn1_w_gu[l], ffn1_w_down[l])
        x = x + 0.5 * rms_norm(h, g[1])
        u = rms_norm(x, g[2])
        (nq, kcm, vcm, ksl, vsl, kwn, vwn, ngate,
         mq, mk, mv, mo, mi, mf) = jnp.split(u @ mix_w_in[l], IN_SPLITS, axis=-1)
        y_nsa = nsa_group(nq, kcm, vcm, ksl, vsl, kwn, vwn, ngate,
                          nsa_cmp_pe[l], nsa_cmp_w1[l], nsa_cmp_w2[l], cos, sin)
        y_mlstm = mlstm_group(mq, mk, mv, mo, mi, mf, mlstm_conv_w[l], mlstm_conv_b[l],
                              mlstm_norm_g[l], mlstm_i_bias[l], mlstm_f_bias[l])
        h = jnp.concatenate([y_nsa, y_mlstm], axis=-1) @ mix_w_out[l]
        x = x + rms_norm(h, g[3])
        h = swiglu(rms_norm(x, g[4]), ffn2_w_gu[l], ffn2_w_down[l])
        x = x + 0.5 * rms_norm(h, g[5])
    return x
```

```python
import math
import numpy as np
import ml_dtypes
import concourse.bass as bass
import concourse.mybir as mybir
from concourse.bass_utils import run_bass_kernel_spmd

F32 = mybir.dt.float32
BF16 = mybir.dt.bfloat16
AF = mybir.ActivationFunctionType
ALU = mybir.AluOpType

D = 1024
S = 2048
NB = 2
T = NB * S
DFF = 2816
NJ = DFF // 128
INC = 3360
EPS = 1e-6
BIG = 30000.0
N_CMP = 127
DEPTH = 2


def _dtsize(dt):
    return 4 if dt == F32 else 2


class Buf:
    __slots__ = ("t", "_w", "_r", "name", "root")

    def __init__(self, t, name="", root=None):
        self.t = t
        self._w = None
        self._r = {}
        self.name = name
        self.root = root

    @property
    def w(self):
        return self.root._w if self.root is not None else self._w

    @w.setter
    def w(self, v):
        if self.root is not None:
            self.root._w = v
        else:
            self._w = v

    @property
    def r(self):
        return self.root._r if self.root is not None else self._r

    @r.setter
    def r(self, v):
        if self.root is not None:
            self.root._r = v
        else:
            self._r = v

    def __getitem__(self, k):
        return self.t[k]


class FW:
    NDMA = 24

    def __init__(self, nc):
        self.nc = nc
        self.eng = {"pe": nc.tensor, "act": nc.scalar, "dve": nc.vector, "pool": nc.gpsimd, "sp": nc.sync}
        self.sem = {k: nc.alloc_semaphore("s_" + k) for k in self.eng}
        self.cnt = {k: 0 for k in self.eng}
        self.waited = {k: {} for k in self.eng}
        self.pending = {k: ([], []) for k in self.eng}
        self.dsem = [nc.alloc_semaphore("d%d" % i) for i in range(2 * self.NDMA)]
        self.dval = [0] * (2 * self.NDMA)
        self.dnext = {"sp": 0, "pool": 0}
        self.nbuf = 0
        self.sb_lo = 16512
        self.sb_hi = 229344
        self.sb_ptr = self.sb_lo
        self.banks = [nc.alloc_psum_tensor("bank%d" % i, [128, 512], F32) for i in range(8)]
        self.bank_root = [Buf(self.banks[i], "bankroot%d" % i) for i in range(8)]

    def sb(self, shape, dt, name=None):
        self.nbuf += 1
        name = (name or "sb") + "_%d" % self.nbuf
        nbytes = int(np.prod(shape[1:])) * _dtsize(dt)
        nbytes = (nbytes + 31) // 32 * 32
        off = self.sb_ptr
        self.sb_ptr += nbytes
        assert self.sb_ptr <= self.sb_hi, "SBUF overflow %s: %d > %d" % (name, self.sb_ptr, self.sb_hi)
        return Buf(self.nc.alloc_sbuf_tensor_at(name, list(shape), dt, offset=off), name)

    def mark(self):
        return self.sb_ptr

    def release(self, m):
        self.barrier()
        self.sb_ptr = m

    def bank(self, i, dt=F32, lo=0, hi=512, name=None):
        ap = self.banks[i][:, lo:hi]
        if dt != F32:
            ap = ap.bitcast(dt)
        return Buf(ap, name or ("bank%d_%d" % (i, lo)), root=self.bank_root[i])

    def dram(self, name, shape, dt, kind="Internal"):
        return Buf(self.nc.dram_tensor(name, list(shape), dt, kind=kind).ap(), name)

    def _wait(self, e, tok):
        if tok is None:
            return
        key, sem, val = tok
        cur = self.waited[e].get(key, 0)
        if cur >= val:
            return
        self.waited[e][key] = val
        self.eng[e].wait_ge(sem, val)

    def _deps(self, e, reads, writes):
        for b in reads:
            self._wait(e, b.w)
        for b in writes:
            self._wait(e, b.w)
            for t in b.r.values():
                self._wait(e, t)

    def _commit(self, tok, reads, writes):
        for b in reads:
            o = b.r.get(tok[0])
            if o is None or o[2] < tok[2]:
                b.r[tok[0]] = tok
        for b in writes:
            b.w = tok
            b.r = {}

    def op(self, e, ins_fn, reads=(), writes=(), inc=True):
        reads = [b for b in reads if b is not None]
        writes = [b for b in writes if b is not None]
        self._deps(e, reads, writes)
        ins = ins_fn(self.eng[e])
        if inc:
            self.cnt[e] += 1
            ins.then_inc(self.sem[e], 1)
            tok = (e, self.sem[e], self.cnt[e])
            pr, pw = self.pending[e]
            self._commit(tok, reads + pr, writes + pw)
            self.pending[e] = ([], [])
        else:
            self.pending[e][0].extend(reads)
            self.pending[e][1].extend(writes)
        return ins

    def dma(self, q, out, in_, reads=(), writes=(), **kw):
        reads = [b for b in reads if b is not None]
        writes = [b for b in writes if b is not None]
        self._deps(q, reads, writes)
        i = self.dnext[q] + (self.NDMA if q == "pool" else 0)
        self.dnext[q] = (self.dnext[q] + 1) % self.NDMA
        key = "d%d" % i
        if self.dval[i] > 0:
            self._wait(q, (key, self.dsem[i], self.dval[i]))
        self.dval[i] += 16
        self.eng[q].dma_start(out=out, in_=in_, **kw).then_inc(self.dsem[i], 16)
        tok = (key, self.dsem[i], self.dval[i])
        self._commit(tok, reads, writes)
        return tok

    def barrier(self):
        toks = [(k, self.sem[k], self.cnt[k]) for k in self.eng if self.cnt[k] > 0]
        toks += [("d%d" % i, self.dsem[i], self.dval[i]) for i in range(2 * self.NDMA) if self.dval[i] > 0]
        for e in self.eng:
            for t in toks:
                self._wait(e, t)

    def finish(self):
        self.barrier()


class Ctx:
    pass


def load_bcast(fw, dst, src_row_ap, src_buf, q="sp"):
    fw.dma(q, dst[:], src_row_ap.partition_broadcast(128), reads=[src_buf], writes=[dst])


def rms_prenorm(fw, cx, xt, gpre, xn, ssq, rs, junk):
    fw.op("act", lambda e: e.activation(out=junk[:], in_=xt[:], func=AF.Square, accum_out=ssq[:]),
          [xt], [junk, ssq])
    fw.op("act", lambda e: e.activation(out=rs[:], in_=ssq[:], func=AF.Sqrt, scale=1.0 / D, bias=EPS),
          [ssq], [rs])
    fw.op("dve", lambda e: e.reciprocal(out=rs[:], in_=rs[:]), [rs], [rs])
    fw.op("dve", lambda e: e.scalar_tensor_tensor(out=xn[:], in0=xt[:], scalar=rs[:, 0:1], in1=gpre[:],
                                                  op0=ALU.mult, op1=ALU.mult), [xt, rs, gpre], [xn])


def transpose_to(fw, cx, src, dstT, dst_cols, pst, nblk=8):
    for k in range(nblk):
        fw.op("pe", lambda e, k=k: e.transpose(out=pst[:, k * 128:(k + 1) * 128], in_=src[:, k * 128:(k + 1) * 128],
                                               identity=cx.ident[:]),
              [src, cx.ident], [pst], inc=(k == nblk - 1))
    fw.op("dve", lambda e: e.tensor_copy(out=dstT[:, 0:nblk, dst_cols],
                                         in_=pst[:, 0:nblk * 128].rearrange("p (k t) -> p k t", k=nblk)),
          [pst], [dstT])


def post_residual(fw, cx, ph0, ph1, xr, gpost, half_scale, ssq2, rs2, junk, tmp):
    fw.op("act", lambda e: e.activation(out=junk[:, 0:512], in_=ph0[:], func=AF.Square, accum_out=ssq2[:, 0:1]),
          [ph0], [junk, ssq2])
    fw.op("act", lambda e: e.activation(out=junk[:, 0:512], in_=ph1[:], func=AF.Square, accum_out=ssq2[:, 1:2]),
          [ph1], [junk, ssq2])
    fw.op("dve", lambda e: e.tensor_tensor(out=rs2[:], in0=ssq2[:, 0:1], in1=ssq2[:, 1:2], op=ALU.add),
          [ssq2], [rs2])
    k = 1.0 / (half_scale * half_scale)
    fw.op("act", lambda e: e.activation(out=rs2[:], in_=rs2[:], func=AF.Sqrt, scale=k / D, bias=k * EPS),
          [rs2], [rs2])
    fw.op("dve", lambda e: e.reciprocal(out=rs2[:], in_=rs2[:]), [rs2], [rs2])
    for hf, ph in ((0, ph0), (1, ph1)):
        sl = slice(hf * 512, (hf + 1) * 512)
        fw.op("dve", lambda e, ph=ph, sl=sl: e.scalar_tensor_tensor(out=tmp[:], in0=ph[:], scalar=rs2[:, 0:1],
                                                                    in1=gpost[:, sl], op0=ALU.mult, op1=ALU.mult),
              [ph, rs2, gpost], [tmp])
        fw.op("dve", lambda e, sl=sl: e.tensor_tensor(out=xr[:, sl], in0=xr[:, sl], in1=tmp[:], op=ALU.add),
              [xr, tmp], [xr])


def ffn_phase(fw, cx, src, dst, w_gu, w_dn, g_all, l, gi_pre, gi_post, ntiles=T // 512):
    nc = fw.nc
    m0 = fw.mark()
    wgu_t = fw.sb([128, 8, 2 * DFF], BF16, "wgu")
    wdn_t = fw.sb([128, NJ, D], BF16, "wdn")
    wgu_g = [Buf(wgu_t.t, "wgu_g%d" % j) for j in range(NJ)]
    wgu_u = [Buf(wgu_t.t, "wgu_u%d" % j) for j in range(NJ)]
    wdn_b = [Buf(wdn_t.t, "wdn%d" % j) for j in range(NJ)]
    gpre = fw.sb([128, D], F32, "gpre")
    gpost = fw.sb([128, D], F32, "gpost")
    xnT = fw.sb([128, 8, 512], BF16, "xnT")
    actT = fw.sb([128, NJ, 512], BF16, "actT")
    actT_j = [Buf(actT.t, "actT%d" % j) for j in range(NJ)]
    xin = [fw.sb([128, D], F32, "xin") for _ in range(2)]
    xres = [fw.sb([128, D], F32, "xres") for _ in range(2)]
    xn = [fw.sb([128, D], BF16, "xn") for _ in range(2)]
    sg = [fw.sb([128, 512], BF16, "sg") for _ in range(2)]
    tmp = [fw.sb([128, 512], F32, "tmp") for _ in range(2)]
    junk = fw.sb([128, D], BF16, "junk")
    ssq = [fw.sb([128, 1], F32, "ssq") for _ in range(2)]
    rs = [fw.sb([128, 1], F32, "rs") for _ in range(2)]
    ssq2 = [fw.sb([128, 2], F32, "ssq2") for _ in range(2)]
    rs2 = [fw.sb([128, 1], F32, "rs2") for _ in range(2)]
    pg = [fw.bank(0), fw.bank(1)]
    pu = [fw.bank(2), fw.bank(3)]
    po = [fw.bank(4), fw.bank(5)]
    pst = [fw.bank(6, BF16), fw.bank(7, BF16)]

    load_bcast(fw, gpre, g_all[l, gi_pre:gi_pre + 1, :], g_all)
    load_bcast(fw, gpost, g_all[l, gi_post:gi_post + 1, :], g_all)
    wsrc = w_gu[l].rearrange("(kc p) n -> p kc n", p=128)
    for j in range(NJ):
        fw.dma("pool", wgu_t[:, :, j * 128:(j + 1) * 128], wsrc[:, :, j * 128:(j + 1) * 128],
               reads=[w_gu], writes=[wgu_g[j]])
        fw.dma("pool", wgu_t[:, :, DFF + j * 128:DFF + (j + 1) * 128], wsrc[:, :, DFF + j * 128:DFF + (j + 1) * 128],
               reads=[w_gu], writes=[wgu_u[j]])
    for j in range(NJ):
        fw.dma("pool", wdn_t[:, j, :], w_dn[l, j * 128:(j + 1) * 128, :], reads=[w_dn], writes=[wdn_b[j]])

    cnt = [0]

    def pre(tile):
        for s in range(4):
            i = cnt[0] % 2
            cnt[0] += 1
            r0 = tile * 512 + s * 128
            fw.dma("sp", xin[i][:], src[r0:r0 + 128, :], reads=[src], writes=[xin[i]])
            rms_prenorm(fw, cx, xin[i], gpre, xn[i], ssq[i], rs[i], junk)
            transpose_to(fw, cx, xn[i], xnT, slice(s * 128, (s + 1) * 128), pst[i])

    pre(0)
    for tile in range(ntiles):
        for j in range(NJ):
            a, b = pg[j % 2], pu[j % 2]
            for kc in range(8):
                fw.op("pe", lambda e, kc=kc, a=a: e.matmul(a[:], lhsT=wgu_t[:, kc, j * 128:(j + 1) * 128],
                                                           rhs=xnT[:, kc, :], start=(kc == 0), stop=(kc == 7)),
                      [wgu_g[j], xnT], [a], inc=(kc == 7))
            for kc in range(8):
                fw.op("pe", lambda e, kc=kc, b=b: e.matmul(b[:], lhsT=wgu_t[:, kc, DFF + j * 128:DFF + (j + 1) * 128],
                                                           rhs=xnT[:, kc, :], start=(kc == 0), stop=(kc == 7)),
                      [wgu_u[j], xnT], [b], inc=(kc == 7))
            s_ = sg[j % 2]
            fw.op("act", lambda e, a=a, s_=s_: e.activation(out=s_[:], in_=a[:], func=AF.Silu), [a], [s_])
            fw.op("dve", lambda e, b=b, s_=s_: e.tensor_tensor(out=actT[:, j, :], in0=b[:], in1=s_[:], op=ALU.mult),
                  [b, s_], [actT_j[j]])
        if tile + 1 < ntiles:
            pre(tile + 1)
        for s in range(4):
            i = s % 2
            r0 = tile * 512 + s * 128
            fw.dma("sp", xres[i][:], src[r0:r0 + 128, :], reads=[src], writes=[xres[i]])
            for hf in range(2):
                for j in range(NJ):
                    fw.op("pe", lambda e, j=j, hf=hf: e.matmul(po[hf][:], lhsT=actT[:, j, s * 128:(s + 1) * 128],
                                                               rhs=wdn_t[:, j, hf * 512:(hf + 1) * 512],
                                                               start=(j == 0), stop=(j == NJ - 1)),
                          [actT_j[j], wdn_b[j]], [po[hf]], inc=(j == NJ - 1))
            post_residual(fw, cx, po[0], po[1], xres[i], gpost, 0.5, ssq2[i], rs2[i], junk, tmp[i])
            fw.dma("sp", dst[r0:r0 + 128, :], xres[i][:], reads=[xres[i]], writes=[dst])
    fw.release(m0)


C_Q, C_KCMP, C_VCMP, C_KSLC, C_VSLC, C_KWIN, C_VWIN, C_GATE = 0, 512, 640, 768, 896, 1024, 1152, 1280
C_MQ, C_MK, C_MV, C_MO, C_MI, C_MF = 1304, 1816, 2328, 2840, 3352, 3356
SCALE = 0.125
MSCALE = 128.0 ** -0.5


def make_uT(fw, cx, src, b, gpre, uT, xin, xn, ssq, rs, junk, pst):
    for s in range(16):
        i = s % 2
        r0 = b * S + s * 128
        fw.dma("sp", xin[i][:], src[r0:r0 + 128, :], reads=[src], writes=[xin[i]])
        rms_prenorm(fw, cx, xin[i], gpre, xn[i], ssq[i], rs[i], junk)
        transpose_to(fw, cx, xn[i], uT, slice(s * 128, (s + 1) * 128), pst[i])


class WStream:
    def __init__(self, fw, w_in, l, nbuf=2):
        self.fw = fw
        self.w_in = w_in
        self.src = w_in[l].rearrange("(kc p) n -> p kc n", p=128)
        self.bufs = [fw.sb([128, 8, 512], BF16, "wbuf") for _ in range(nbuf)]
        self.i = 0

    def load(self, c0, c1):
        wb = self.bufs[self.i % len(self.bufs)]
        self.i += 1
        self.fw.dma("pool", wb[:, :, 0:c1 - c0], self.src[:, :, c0:c1], reads=[self.w_in], writes=[wb])
        return wb


def proj_fm(fw, wb, off, M, uT, tile, ps):
    for kc in range(8):
        fw.op("pe", lambda e, kc=kc: e.matmul(ps[0:M, :], lhsT=wb[:, kc, off:off + M],
                                              rhs=uT[:, kc, tile * 512:(tile + 1) * 512],
                                              start=(kc == 0), stop=(kc == 7)),
              [wb, uT], [ps], inc=(kc == 7))


def proj_tm(fw, wb, off, N, uT, s, ps):
    for kc in range(8):
        fw.op("pe", lambda e, kc=kc: e.matmul(ps[:, 0:N], lhsT=uT[:, kc, s * 128:(s + 1) * 128],
                                              rhs=wb[:, kc, off:off + N], start=(kc == 0), stop=(kc == 7)),
              [wb, uT], [ps], inc=(kc == 7))


def rope_inplace(fw, cx, dst_buf, dst_ap64, dst_ap16, pq, tile, psw, t1, t2):
    cols = slice(tile * 512, (tile + 1) * 512)
    fw.op("pe", lambda e: e.matmul(psw[0:16, :], lhsT=cx.perm[0:64, 0:16], rhs=dst_ap64, start=True, stop=True),
          [cx.perm, dst_buf], [psw])
    fw.op("dve", lambda e: e.tensor_tensor(out=t1[:], in0=psw[0:16, :], in1=cx.ropeS[:, cols], op=ALU.mult),
          [psw, cx.ropeS], [t1])
    fw.op("dve", lambda e: e.tensor_tensor(out=t2[:], in0=pq[0:16, :], in1=cx.ropeC[:, cols], op=ALU.mult),
          [pq, cx.ropeC], [t2])
    fw.op("dve", lambda e: e.tensor_tensor(out=dst_ap16, in0=t1[:], in1=t2[:], op=ALU.add),
          [t1, t2], [dst_buf])


def mixer_phase(fw, cx, src, dst, W, C, l, dbg=None):
    nc = fw.nc
    w_in = W["mix_w_in"]
    mP = fw.mark()
    gpre = fw.sb([128, D], F32, "gpre")
    gpost = fw.sb([128, D], F32, "gpost")
    load_bcast(fw, gpre, W["norm_g"][l, 2:3, :], W["norm_g"])
    load_bcast(fw, gpost, W["norm_g"][l, 3:4, :], W["norm_g"])
    ibfb = fw.sb([128, 8], F32, "ibfb")
    fw.dma("sp", ibfb[:, 0:4], W["mlstm_i_bias"][l:l + 1, :].partition_broadcast(128), reads=[W["mlstm_i_bias"]], writes=[ibfb])
    fw.dma("sp", ibfb[:, 4:8], W["mlstm_f_bias"][l:l + 1, :].partition_broadcast(128), reads=[W["mlstm_f_bias"]], writes=[ibfb])
    mng = fw.sb([128, 512], F32, "mng")
    load_bcast(fw, mng, W["mlstm_norm_g"][l:l + 1, :], W["mlstm_norm_g"])
    cw = fw.sb([128, 8, 4], F32, "cw")
    cb = fw.sb([128, 8], F32, "cb")
    for c in range(8):
        fw.dma("sp", cw[:, c, :], W["mlstm_conv_w"][l, :, c * 128:(c + 1) * 128].rearrange("j p -> p j"),
               reads=[W["mlstm_conv_w"]], writes=[cw], allow_slow_non_contiguous=True)
        fw.dma("sp", cb[:, c:c + 1], W["mlstm_conv_b"][l:l + 1, c * 128:(c + 1) * 128].rearrange("o p -> p o"),
               reads=[W["mlstm_conv_b"]], writes=[cb], allow_slow_non_contiguous=True)
    w2sb = fw.sb([128, 2, 64], BF16, "w2sb")
    fw.dma("pool", w2sb[:], W["nsa_cmp_w2"][l].rearrange("k h d -> h k d"), reads=[W["nsa_cmp_w2"]], writes=[w2sb])
    cbias = fw.sb([128, 2], F32, "cbias")
    mb = fw.mark()
    w1r = fw.sb([128, 2, 16, 128], BF16, "w1r")
    pef = fw.sb([128, 2, 16], BF16, "pef")
    for kv in range(2):
        fw.dma("pool", w1r[:, kv], W["nsa_cmp_w1"][l, kv].rearrange("(p c) h -> p c h", c=16),
               reads=[W["nsa_cmp_w1"]], writes=[w1r])
        fw.dma("pool", pef[:, kv], W["nsa_cmp_pe"][l, kv].rearrange("l d -> (l d)").rearrange("(p c) -> p c", c=16),
               reads=[W["nsa_cmp_pe"]], writes=[pef])
    pb = fw.bank(0)
    for kv in range(2):
        for c in range(16):
            fw.op("pe", lambda e, kv=kv, c=c: e.matmul(pb[:, kv:kv + 1], lhsT=w1r[:, kv, c, :], rhs=pef[:, kv, c:c + 1],
                                                       start=(c == 0), stop=(c == 15)),
                  [w1r, pef], [pb], inc=(c == 15))
    fw.op("dve", lambda e: e.tensor_copy(out=cbias[:], in_=pb[:, 0:2]), [pb], [cbias])
    fw.release(mb)

    for b in range(NB):
        mB = fw.mark()
        yT_nsa = fw.sb([128, 4, S], BF16, "yTnsa")
        m1 = fw.mark()
        qaug = [fw.sb([96, 16, 4, 128], BF16, "qaug") for _ in range(2)]
        kslc = fw.sb([96, 2, S], BF16, "kslc")
        kwin = fw.sb([64, 2, S], BF16, "kwin")
        vslc = fw.sb([128, 16, 2, 65], BF16, "vslc")
        vwin = fw.sb([128, 16, 2, 65], BF16, "vwin")
        sig = fw.sb([128, 16, 24], F32, "sig")
        kcT = fw.sb([64, 2, 128], BF16, "kcT")
        vcx = fw.sb([128, 2, 97], BF16, "vcx")
        m1b = fw.mark()
        uT = fw.sb([128, 8, S], BF16, "uT")
        ws = WStream(fw, w_in, l)
        kvcmp = fw.sb([128, 2, S], BF16, "kvcmp")
        w1sb = fw.sb([128, 2, 32, 128], BF16, "w1sb")
        ropeC = fw.sb([16, S], F32, "ropeC")
        ropeS = fw.sb([16, S], F32, "ropeS")
        cx.ropeC, cx.ropeS = ropeC, ropeS
        xin = [fw.sb([128, D], F32, "xin") for _ in range(2)]
        xn = [fw.sb([128, D], BF16, "xn") for _ in range(2)]
        junk = fw.sb([128, D], BF16, "junk")
        ssq = [fw.sb([128, 1], F32, "ssq") for _ in range(2)]
        rs = [fw.sb([128, 1], F32, "rs") for _ in range(2)]
        t1 = [fw.sb([16, 512], F32, "t1") for _ in range(2)]
        t2 = [fw.sb([16, 512], F32, "t2") for _ in range(2)]
        gel = [fw.sb([128, 128], BF16, "gel") for _ in range(2)]
        fw.dma("sp", ropeC[:], C["c_rope_c"][:, :], reads=[C["c_rope_c"]], writes=[ropeC])
        fw.dma("sp", ropeS[:], C["c_rope_s"][:, :], reads=[C["c_rope_s"]], writes=[ropeS])
        for kv in range(2):
            for g in range(2):
                fw.dma("pool", w1sb[g * 64:(g + 1) * 64, kv], W["nsa_cmp_w1"][l, kv].rearrange("(l d) h -> d l h", d=64),
                       reads=[W["nsa_cmp_w1"]], writes=[w1sb])
        fw.dma("pool", kslc[64:96, 0, :], C["c_E"][:, :], reads=[C["c_E"]], writes=[kslc])
        fw.dma("pool", kslc[64:96, 1, :], C["c_E"][:, :], reads=[C["c_E"]], writes=[kslc])
        for g in range(2):
            fw.dma("pool", vcx[:, g, 64:97], C["c_ovl"][:, :], reads=[C["c_ovl"]], writes=[vcx])
            fw.op("pool", lambda e, g=g: e.memset(qaug[g][64:96], 0.0), [], [qaug[g]])
        fw.op("pool", lambda e: e.memset(vslc[:, :, :, 64:65], 1.0), [], [vslc])
        fw.op("pool", lambda e: e.memset(vwin[:, :, :, 64:65], 1.0), [], [vwin])
        pst = [fw.bank(6, BF16), fw.bank(7, BF16)]
        make_uT(fw, cx, src, b, gpre, uT, xin, xn, ssq, rs, junk, pst)
        pq = [fw.bank(0), fw.bank(1)]
        psw = [fw.bank(2), fw.bank(3)]
        ptm = [fw.bank(4), fw.bank(5)]
        k = 0
        wb = ws.load(C_Q, C_Q + 512)
        for h in range(8):
            g, hh = h // 4, h % 4
            for tile in range(4):
                p = pq[k % 2]
                proj_fm(fw, wb, h * 64, 64, uT, tile, p)
                d64 = qaug[g][0:64, tile * 4:(tile + 1) * 4, hh, :]
                d16 = qaug[g][0:16, tile * 4:(tile + 1) * 4, hh, :]
                fw.op("act", lambda e, p=p, d64=d64: e.activation(out=d64, in_=p[0:64, :].rearrange("p (a q) -> p a q", a=4),
                                                                  func=AF.Copy), [p], [qaug[g]])
                _rope(fw, cx, qaug[g], d64, d16, p, tile, psw[k % 2], t1[k % 2], t2[k % 2], four=True)
                k += 1
        wb = ws.load(C_KCMP, C_KCMP + 512)
        for tile in range(4):
            cols = slice(tile * 512, (tile + 1) * 512)
            for kv in range(2):
                p = pq[k % 2]
                proj_fm(fw, wb, kv * 128, 128, uT, tile, p)
                fw.op("act", lambda e, p=p, kv=kv: e.activation(out=kvcmp[:, kv, cols], in_=p[:, :], func=AF.Copy),
                      [p], [kvcmp])
                k += 1
            for g in range(2):
                p = pq[k % 2]
                proj_fm(fw, wb, 256 + g * 64, 64, uT, tile, p)
                d64 = kslc[0:64, g, cols]
                d16 = kslc[0:16, g, cols]
                fw.op("act", lambda e, p=p, d64=d64: e.activation(out=d64, in_=p[0:64, :], func=AF.Copy), [p], [kslc])
                _rope(fw, cx, kslc, d64, d16, p, tile, psw[k % 2], t1[k % 2], t2[k % 2])
                k += 1
        for s in range(16):
            p = ptm[s % 2]
            proj_tm(fw, wb, 384, 128, uT, s, p)
            fw.op("act", lambda e, p=p, s=s: e.activation(out=vslc[:, s, :, 0:64],
                                                          in_=p[:, 0:128].rearrange("p (g d) -> p g d", g=2), func=AF.Copy),
                  [p], [vslc])
        wb = ws.load(C_KWIN, C_KWIN + 280)
        for tile in range(4):
            cols = slice(tile * 512, (tile + 1) * 512)
            for g in range(2):
                p = pq[k % 2]
                proj_fm(fw, wb, g * 64, 64, uT, tile, p)
                d64 = kwin[0:64, g, cols]
                d16 = kwin[0:16, g, cols]
                fw.op("act", lambda e, p=p, d64=d64: e.activation(out=d64, in_=p[0:64, :], func=AF.Copy), [p], [kwin])
                _rope(fw, cx, kwin, d64, d16, p, tile, psw[k % 2], t1[k % 2], t2[k % 2])
                k += 1
        for s in range(16):
            p = ptm[s % 2]
            proj_tm(fw, wb, 128, 152, uT, s, p)
            fw.op("act", lambda e, p=p, s=s: e.activation(out=vwin[:, s, :, 0:64],
                                                          in_=p[:, 0:128].rearrange("p (g d) -> p g d", g=2), func=AF.Copy),
                  [p], [vwin])
            fw.op("act", lambda e, p=p, s=s: e.activation(out=sig[:, s, :], in_=p[:, 128:152], func=AF.Sigmoid),
                  [p], [sig])
        for kv in range(2):
            for g in range(2):
                p = pq[k % 2]
                gl = gel[k % 2]
                for li in range(32):
                    fw.op("pe", lambda e, li=li, p=p: e.matmul(p[:, 0:127], lhsT=w1sb[g * 64:(g + 1) * 64, kv, li, :],
                                                               rhs=kvcmp[g * 64:(g + 1) * 64, kv, li:li + 2017:16],
                                                               start=(li == 0), stop=(li == 31)),
                          [w1sb, kvcmp], [p], inc=(li == 31))
                fw.op("act", lambda e, p=p, gl=gl: e.activation(out=gl[:, 0:127], in_=p[:, 0:127], func=AF.Gelu_apprx_tanh,
                                                                bias=cbias[:, kv:kv + 1]), [p, cbias], [gl])
                p2 = psw[k % 2]
                if kv == 0:
                    fw.op("pe", lambda e, p2=p2, gl=gl: e.matmul(p2[0:64, 0:127], lhsT=w2sb[:, 0, :], rhs=gl[:, 0:127],
                                                                 start=True, stop=True), [w2sb, gl], [p2])
                    fw.op("dve", lambda e, p2=p2: e.tensor_copy(out=kcT[0:64, g, 0:127], in_=p2[0:64, 0:127]), [p2], [kcT])
                else:
                    fw.op("pe", lambda e, p2=p2, gl=gl: e.matmul(p2[0:127, 0:64], lhsT=gl[:, 0:127], rhs=w2sb[:, 1, :],
                                                                 start=True, stop=True), [w2sb, gl], [p2])
                    fw.op("dve", lambda e, p2=p2: e.tensor_copy(out=vcx[0:127, g, 0:64], in_=p2[0:127, 0:64]), [p2], [vcx])
                k += 1
        fw.release(m1b)
        nsa_attention(fw, cx, C, qaug, kslc, kwin, vslc, vwin, sig, kcT, vcx, yT_nsa)
        if dbg is not None and b == 0:
            tcp = fw.sb([128, 4, S], F32, "dbgcp")
            fw.op("dve", lambda e: e.tensor_copy(out=tcp[:], in_=yT_nsa[:]), [yT_nsa], [tcp])
            fw.dma("sp", dbg["yT_nsa"][:, :, :], tcp[:], reads=[tcp], writes=[dbg["yT_nsa"]])
        fw.release(m1)
        mlstm_and_out(fw, cx, src, dst, W, C, l, b, gpre, gpost, ibfb, mng, cw, cb, yT_nsa, dbg)
        fw.release(mB)
    fw.release(mP)


def _rope(fw, cx, dst_buf, d64, d16, pq, tile, psw, t1, t2, four=False):
    cols = slice(tile * 512, (tile + 1) * 512)
    fw.op("pe", lambda e: e.matmul(psw[0:16, :].rearrange("p (a q) -> p a q", a=4) if four else psw[0:16, :],
                                   lhsT=cx.perm[0:64, 0:16], rhs=d64, start=True, stop=True),
          [cx.perm, dst_buf], [psw])
    fw.op("dve", lambda e: e.tensor_tensor(out=t1[:], in0=psw[0:16, :], in1=cx.ropeS[:, cols], op=ALU.mult),
          [psw, cx.ropeS], [t1])
    fw.op("dve", lambda e: e.tensor_tensor(out=t2[:], in0=pq[0:16, :], in1=cx.ropeC[:, cols], op=ALU.mult),
          [pq, cx.ropeC], [t2])
    o = d16
    a, b_ = (t1[:].rearrange("p (a q) -> p a q", a=4), t2[:].rearrange("p (a q) -> p a q", a=4)) if four else (t1[:], t2[:])
    fw.op("dve", lambda e: e.tensor_tensor(out=o, in0=a, in1=b_, op=ALU.add), [t1, t2], [dst_buf])


def nsa_attention(fw, cx, C, qaug, kslc, kwin, vslc, vwin, sig, kcT, vcx, yT_nsa):
    m = fw.mark()
    causal = fw.sb([128, 128], BF16, "causal")
    anti = fw.sb([128, 128], BF16, "anti")
    cmpneg = fw.sb([128, S], BF16, "cmpneg")
    addm = fw.sb([128, 16, 32], F32, "addm")
    forced = fw.sb([128, 16, 32], F32, "forced")
    fw.dma("pool", causal[:], C["c_causal"][:, :], reads=[C["c_causal"]], writes=[causal])
    fw.dma("pool", anti[:], C["c_anti"][:, :], reads=[C["c_anti"]], writes=[anti])
    fw.dma("pool", cmpneg[:], C["c_cmpneg"][:, :], reads=[C["c_cmpneg"]], writes=[cmpneg])
    fw.dma("sp", addm[:], C["c_addm"].t.rearrange("(c p) j -> p c j", p=128), reads=[C["c_addm"]], writes=[addm])
    fw.dma("sp", forced[:], C["c_forced"].t.rearrange("(c p) j -> p c j", p=128), reads=[C["c_forced"]], writes=[forced])
    pT = [fw.sb([128, 512], BF16, "pT") for _ in range(3)]
    l4 = [fw.sb([128, 4], F32, "l4") for _ in range(3)]
    cf = [fw.sb([128, 4], F32, "cf") for _ in range(3)]
    imp = fw.sb([128, 32], F32, "imp")
    impn = fw.sb([128, 32], F32, "impn")
    imp2 = fw.sb([128, 32], F32, "imp2")
    m8 = fw.sb([128, 16], F32, "m8")
    selb = fw.sb([128, 32], F32, "selb")
    negsel = fw.sb([128, 32], BF16, "negsel")
    yacc = fw.sb([128, 4, 64], F32, "yacc")
    ytmp = fw.sb([128, 4, 64], F32, "ytmp")
    yb = [fw.sb([128, 512], BF16, "yb") for _ in range(2)]
    psc = [fw.bank(0), fw.bank(1), fw.bank(2)]
    po_c = fw.bank(3, F32, 0, 388)
    po_s = fw.bank(4, F32, 0, 260)
    po_w = fw.bank(5, F32, 0, 260)
    ptr = fw.bank(6, BF16)
    pyt = fw.bank(7, BF16)
    po_c3 = po_c.t.rearrange("p (h c) -> p h c", h=4)
    po_s3 = po_s.t.rearrange("p (h c) -> p h c", h=4)
    po_w3 = po_w.t.rearrange("p (h c) -> p h c", h=4)
    sci = [0]

    def scores(lhsT_ap, lhs_buf, K, M, g, qt, mask_buf, mask_ap):
        i = sci[0] % 3
        sci[0] += 1
        ps, pt = psc[i], pT[i]
        ps3 = ps[0:M, :].rearrange("p (h q) -> p h q", h=4)
        fw.op("pe", lambda e: e.matmul(ps3, lhsT=lhsT_ap, rhs=qaug[g][0:K, qt], start=True, stop=(mask_buf is None)),
              [lhs_buf, qaug[g]], [ps], inc=(mask_buf is None))
        if mask_buf is not None:
            fw.op("pe", lambda e: e.matmul(ps3, lhsT=cx.ident[0:M, 0:M],
                                           rhs=mask_ap.unsqueeze(1).broadcast_to([M, 4, 128]), start=False, stop=True),
                  [cx.ident, mask_buf], [ps])
        return ps, pt

    def expo(ps, pt, M):
        fw.op("act", lambda e: e.activation(out=pt[0:M, :], in_=ps[0:M, :], func=AF.Exp, scale=SCALE), [ps], [pt])

    def pv(pt, M, po_buf, po3, v_ap, v_buf, first, last):
        for hh in range(4):
            fw.op("pe", lambda e, hh=hh: e.matmul(po3[:, hh, :], lhsT=pt[0:M, hh * 128:(hh + 1) * 128], rhs=v_ap,
                                                  start=(first and hh == 0), stop=(last and hh == 3),
                                                  skip_group_check=True),
                  [pt, v_buf], [po_buf], inc=(hh == 3))

    def coefs(br, po_buf, po3, g, qt):
        fw.op("dve", lambda e: e.tensor_scalar(out=l4[br][:], in0=po3[:, :, 64], scalar1=1e-30, scalar2=None, op0=ALU.max),
              [po_buf], [l4[br]])
        fw.op("dve", lambda e: e.reciprocal(out=l4[br][:], in_=l4[br][:]), [l4[br]], [l4[br]])
        c0 = br * 8 + g * 4
        fw.op("dve", lambda e: e.tensor_tensor(out=cf[br][:], in0=l4[br][:], in1=sig[:, qt, c0:c0 + 4], op=ALU.mult),
              [l4[br], sig], [cf[br]])

    def after_cmp(g, qt):
        coefs(0, po_c, po_c3, g, qt)
        if qt >= 8:
            fw.op("dve", lambda e: e.tensor_scalar(out=imp[:], in0=po_c3[:, 0, 65:97], scalar1=l4[0][:, 0:1],
                                                   scalar2=None, op0=ALU.mult), [po_c, l4[0]], [imp])
            for hh in range(1, 4):
                fw.op("dve", lambda e, hh=hh: e.scalar_tensor_tensor(out=imp[:], in0=po_c3[:, hh, 65:97],
                                                                     scalar=l4[0][:, hh:hh + 1], in1=imp[:],
                                                                     op0=ALU.mult, op1=ALU.add),
                      [po_c, l4[0], imp], [imp])
            fw.op("dve", lambda e: e.tensor_tensor(out=impn[:], in0=imp[:], in1=addm[:, qt, :], op=ALU.add),
                  [imp, addm], [impn])
            fw.op("dve", lambda e: e.max(out=m8[:, 0:8], in_=impn[:]), [impn], [m8])
            fw.op("dve", lambda e: e.match_replace(out=imp2[:], in_to_replace=m8[:, 0:8], in_values=impn[:],
                                                   imm_value=-1e30), [impn, m8], [imp2])
            fw.op("dve", lambda e: e.max(out=m8[:, 8:16], in_=imp2[:]), [imp2], [m8])
            fw.op("dve", lambda e: e.tensor_scalar(out=selb[:], in0=impn[:], scalar1=m8[:, 12:13], scalar2=None,
                                                   op0=ALU.is_ge), [impn, m8], [selb])
            fw.op("dve", lambda e: e.tensor_tensor(out=selb[:], in0=selb[:], in1=forced[:, qt, :], op=ALU.max),
                  [selb, forced], [selb])
            fw.op("dve", lambda e: e.tensor_scalar(out=negsel[:], in0=selb[:], scalar1=-1.0, scalar2=BIG,
                                                   op0=ALU.add, op1=ALU.mult), [selb], [negsel])
            fw.op("pe", lambda e: e.transpose(out=ptr[0:32, 0:128], in_=negsel[:, :], identity=cx.ident[:]),
                  [negsel, cx.ident], [ptr])
            fw.op("dve", lambda e: e.tensor_copy(out=qaug[g][64:96, qt],
                                                 in_=ptr[0:32, 0:128].unsqueeze(1).broadcast_to([32, 4, 128])),
                  [ptr], [qaug[g]])
        fw.op("dve", lambda e: e.tensor_tensor(out=yacc[:], in0=po_c3[:, :, 0:64],
                                               in1=cf[0][:].unsqueeze(2).broadcast_to([128, 4, 64]), op=ALU.mult),
              [po_c, cf[0]], [yacc])

    def after_win(g, qt):
        coefs(2, po_w, po_w3, g, qt)
        fw.op("dve", lambda e: e.tensor_tensor(out=ytmp[:], in0=po_w3[:, :, 0:64],
                                               in1=cf[2][:].unsqueeze(2).broadcast_to([128, 4, 64]), op=ALU.mult),
              [po_w, cf[2]], [ytmp])
        fw.op("dve", lambda e: e.tensor_tensor(out=yacc[:], in0=yacc[:], in1=ytmp[:], op=ALU.add), [yacc, ytmp], [yacc])

    def after_sel(g, qt):
        ybq = yb[qt % 2]
        coefs(1, po_s, po_s3, g, qt)
        fw.op("dve", lambda e: e.tensor_tensor(out=ytmp[:], in0=po_s3[:, :, 0:64],
                                               in1=cf[1][:].unsqueeze(2).broadcast_to([128, 4, 64]), op=ALU.mult),
              [po_s, cf[1]], [ytmp])
        fw.op("dve", lambda e: e.tensor_tensor(out=ybq[:, g * 256:(g + 1) * 256].rearrange("p (h d) -> p h d", h=4),
                                               in0=yacc[:], in1=ytmp[:], op=ALU.add), [yacc, ytmp], [ybq])
        if g == 1:
            transpose_to(fw, cx, ybq, yT_nsa, slice(qt * 128, (qt + 1) * 128), pyt, nblk=4)

    jobs = []
    for qt in range(16):
        for g in range(2):
            jobs.append(((kcT[0:64, g, 0:127], kcT, 64, 127, g, qt, cmpneg, cmpneg[0:127, qt * 128:(qt + 1) * 128]),
                         (127, po_c, po_c3, vcx[0:127, g, :], vcx, True, True),
                         (lambda g=g, qt=qt: after_cmp(g, qt))))
            k0 = max(0, qt - 4)
            for kc in range(k0, qt + 1):
                ksl = slice(kc * 128, (kc + 1) * 128)
                if kc == qt:
                    mb, ma = causal, causal[:, :]
                elif kc == qt - 4:
                    mb, ma = anti, anti[:, :]
                else:
                    mb, ma = None, None
                jobs.append(((kwin[0:64, g, ksl], kwin, 64, 128, g, qt, mb, ma),
                             (128, po_w, po_w3, vwin[:, kc, g, :], vwin, kc == k0, kc == qt),
                             (lambda g=g, qt=qt: after_win(g, qt)) if kc == qt else None))
            for kc in range(qt + 1):
                ksl = slice(kc * 128, (kc + 1) * 128)
                jobs.append(((kslc[0:96, g, ksl], kslc, 96, 128, g, qt,
                              causal if kc == qt else None, causal[:, :] if kc == qt else None),
                             (128, po_s, po_s3, vslc[:, kc, g, :], vslc, kc == 0, kc == qt),
                             (lambda g=g, qt=qt: after_sel(g, qt)) if kc == qt else None))
    nxt = scores(*jobs[0][0])
    for i, (sa, pa, hook) in enumerate(jobs):
        ps, pt = nxt
        expo(ps, pt, pa[0])
        if i + 1 < len(jobs):
            nxt = scores(*jobs[i + 1][0])
        pv(pt, *pa)
        if hook is not None:
            hook()
    fw.release(m)


def mlstm_and_out(fw, cx, src, dst, W, C, l, b, gpre, gpost, ibfb, mng, cw, cb, yT_nsa, dbg):
    w_in = W["mix_w_in"]
    m3 = fw.mark()
    mqT = fw.sb([128, 4, S], BF16, "mqT")
    mkT = fw.sb([128, 4, S], BF16, "mkT")
    mv = fw.sb([128, 16, 4, 129], BF16, "mv")
    sigo = fw.sb([128, 16, 512], BF16, "sigo")
    mif = fw.sb([128, 16, 8], F32, "mif")
    m3b = fw.mark()
    uT = fw.sb([128, 8, S], BF16, "uT")
    ws = WStream(fw, w_in, l)
    xin = [fw.sb([128, D], F32, "xin") for _ in range(2)]
    xn = [fw.sb([128, D], BF16, "xn") for _ in range(2)]
    junk = fw.sb([128, D], BF16, "junk")
    ssq = [fw.sb([128, 1], F32, "ssq") for _ in range(2)]
    rs = [fw.sb([128, 1], F32, "rs") for _ in range(2)]
    raw = [fw.sb([128, 515], F32, "raw") for _ in range(3)]
    acc = [fw.sb([128, 512], F32, "acc") for _ in range(2)]
    pst = [fw.bank(6, BF16), fw.bank(7, BF16)]
    fw.op("pool", lambda e: e.memset(mv[:, :, :, 128:129], 1.0), [], [mv])
    make_uT(fw, cx, src, b, gpre, uT, xin, xn, ssq, rs, junk, pst)
    pq = [fw.bank(0), fw.bank(1)]
    ptm = [fw.bank(4), fw.bank(5)]
    k = 0
    ri = 0
    for (coff, dstT, cbase) in ((C_MQ, mqT, 0), (C_MK, mkT, 4)):
        wb = ws.load(coff, coff + 512)
        for h in range(4):
            c = cbase + h
            prev = None
            for tile in range(4):
                r = raw[ri % 3]
                ri += 1
                if prev is None:
                    fw.op("pool", lambda e, r=r: e.memset(r[:, 0:3], 0.0), [], [r])
                else:
                    fw.op("pool", lambda e, r=r, prev=prev: e.tensor_copy(out=r[:, 0:3], in_=prev[:, 512:515]), [prev], [r])
                p = pq[k % 2]
                a = acc[k % 2]
                k += 1
                proj_fm(fw, wb, h * 128, 128, uT, tile, p)
                fw.op("act", lambda e, p=p, r=r: e.activation(out=r[:, 3:515], in_=p[:, :], func=AF.Copy), [p], [r])
                fw.op("dve", lambda e, r=r, a=a: e.tensor_scalar(out=a[:], in0=r[:, 3:515], scalar1=cw[:, c, 3:4],
                                                                  scalar2=cb[:, c:c + 1], op0=ALU.mult, op1=ALU.add),
                      [r, cw, cb], [a])
                for j in (2, 1, 0):
                    fw.op("dve", lambda e, r=r, a=a, j=j: e.scalar_tensor_tensor(out=a[:], in0=r[:, j:j + 512],
                                                                                  scalar=cw[:, c, j:j + 1], in1=a[:],
                                                                                  op0=ALU.mult, op1=ALU.add),
                          [r, cw, a], [a])
                fw.op("act", lambda e, a=a, tile=tile: e.activation(out=dstT[:, h, tile * 512:(tile + 1) * 512], in_=a[:],
                                                                    func=AF.Silu), [a], [dstT])
                prev = r
    wb = ws.load(C_MV, C_MV + 512)
    for s in range(16):
        p = ptm[s % 2]
        proj_tm(fw, wb, 0, 512, uT, s, p)
        fw.op("act", lambda e, p=p, s=s: e.activation(out=mv[:, s, :, 0:128], in_=p[:, :].rearrange("p (h d) -> p h d", h=4),
                                                      func=AF.Copy), [p], [mv])
    wb = ws.load(C_MO, C_MO + 512)
    for s in range(16):
        p = ptm[s % 2]
        proj_tm(fw, wb, 0, 512, uT, s, p)
        fw.op("act", lambda e, p=p, s=s: e.activation(out=sigo[:, s, :], in_=p[:, :], func=AF.Sigmoid), [p], [sigo])
    wb = ws.load(C_MI, C_MI + 8)
    for s in range(16):
        p = ptm[s % 2]
        proj_tm(fw, wb, 0, 8, uT, s, p)
        fw.op("dve", lambda e, p=p, s=s: e.tensor_copy(out=mif[:, s, :], in_=p[:, 0:8]), [p], [mif])
    fw.release(m3b)
    wout = fw.sb([128, 8, D], BF16, "wout")
    fw.dma("pool", wout[:], W["mix_w_out"][l].rearrange("(kc p) n -> p kc n", p=128), reads=[W["mix_w_out"]], writes=[wout])
    tri = fw.sb([128, 128], BF16, "tri")
    trif = fw.sb([128, 128], F32, "trif")
    onesf = fw.sb([128, 128], F32, "onesf")
    fw.dma("pool", tri[:], C["c_tri"][:, :], reads=[C["c_tri"]], writes=[tri])
    fw.dma("sp", trif[:], C["c_tri"][:, :], reads=[C["c_tri"]], writes=[trif])
    fw.op("pool", lambda e: e.memset(onesf[:], 1.0), [], [onesf])
    Cf = fw.sb([128, 4, 129], F32, "Cf")
    Cb = [fw.sb([128, 4, 129], BF16, "Cb") for _ in range(2)]
    fw.op("pool", lambda e: e.memset(Cf[:], 0.0), [], [Cf])
    z = fw.sb([128, 4], F32, "z")
    e1 = fw.sb([128, 4], F32, "e1")
    sp = fw.sb([128, 4], F32, "sp")
    av = fw.sb([128, 4], F32, "av")
    a2 = fw.sb([128, 4], F32, "a2")
    es = fw.sb([128, 4], F32, "es")
    wk = fw.sb([128, 4], F32, "wk")
    ebt = fw.sb([128, 4], F32, "ebt")
    EB = fw.sb([128, 4], F32, "EB")
    dn = fw.sb([128, 4], F32, "dn")
    coef = fw.sb([128, 4], F32, "coef")
    ssq4 = fw.sb([128, 4], F32, "ssq4")
    rstd4 = fw.sb([128, 4], F32, "rstd4")
    PT = [fw.sb([128, 128], BF16, "PT") for _ in range(4)]
    k2 = [fw.sb([128, 128], BF16, "k2") for _ in range(4)]
    hh = fw.sb([128, 4, 128], F32, "hh")
    junk2 = fw.sb([128, 128], BF16, "junk2")
    junk = fw.sb([128, 512], BF16, "junk")
    ymf = fw.sb([128, 512], F32, "ymf")
    ymb = fw.sb([128, 512], BF16, "ymb")
    yTm = fw.sb([128, 4, 128], BF16, "yTm")
    xres = [fw.sb([128, D], F32, "xres") for _ in range(2)]
    tmp = fw.sb([128, 512], F32, "tmp")
    ssq2 = fw.sb([128, 2], F32, "ssq2")
    rs2 = fw.sb([128, 1], F32, "rs2")
    psT = [fw.bank(0, F32, h * 128, (h + 1) * 128) for h in range(4)]
    pk = [fw.bank(1, BF16, h * 64, (h + 1) * 64) for h in range(4)]
    pyt = fw.bank(1, BF16, 256, 512)
    pcs = fw.bank(2, F32, 384, 392)
    pdC = [fw.bank(2, F32, 0, 129), fw.bank(2, F32, 129, 258), fw.bank(3, F32, 0, 129), fw.bank(3, F32, 129, 258)]
    pnum = [fw.bank(4, F32, 0, 129), fw.bank(4, F32, 129, 258), fw.bank(5, F32, 0, 129), fw.bank(5, F32, 129, 258)]
    ph = [fw.bank(6), fw.bank(7)]
    LN_MS = math.log(MSCALE)
    for c in range(16):
        cc = slice(c * 128, (c + 1) * 128)
        i = c % 2
        r0 = b * S + c * 128
        fw.dma("sp", xres[i][:], src[r0:r0 + 128, :], reads=[src], writes=[xres[i]])
        fw.op("dve", lambda e: e.tensor_tensor(out=z[:], in0=mif[:, c, 4:8], in1=ibfb[:, 4:8], op=ALU.add), [mif, ibfb], [z])
        fw.op("act", lambda e: e.activation(out=e1[:], in_=z[:], func=AF.Exp, scale=-1.0), [z], [e1])
        fw.op("act", lambda e: e.activation(out=sp[:], in_=e1[:], func=AF.Ln, bias=1.0), [e1], [sp])
        fw.op("pe", lambda e: e.matmul(pcs[:, 0:4], lhsT=trif[:], rhs=sp[:], start=True, stop=True), [trif, sp], [pcs], inc=False)
        fw.op("pe", lambda e: e.matmul(pcs[:, 4:8], lhsT=onesf[:], rhs=sp[:], start=True, stop=True), [onesf, sp], [pcs])
        fw.op("dve", lambda e: e.tensor_tensor(out=av[:], in0=mif[:, c, 0:4], in1=ibfb[:, 0:4], op=ALU.add), [mif, ibfb], [av])
        fw.op("dve", lambda e: e.tensor_tensor(out=av[:], in0=av[:], in1=pcs[:, 0:4], op=ALU.add), [av, pcs], [av])
        fw.op("act", lambda e: e.activation(out=es[:], in_=av[:], func=AF.Exp), [av], [es])
        fw.op("dve", lambda e: e.tensor_tensor(out=a2[:], in0=av[:], in1=pcs[:, 4:8], op=ALU.subtract), [av, pcs], [a2])
        fw.op("act", lambda e: e.activation(out=wk[:], in_=a2[:], func=AF.Exp), [a2], [wk])
        fw.op("act", lambda e: e.activation(out=ebt[:], in_=pcs[:, 0:4], func=AF.Exp, scale=-1.0, bias=LN_MS), [pcs], [ebt])
        fw.op("act", lambda e: e.activation(out=EB[:], in_=pcs[:, 4:8], func=AF.Exp, scale=-1.0), [pcs], [EB])
        for h in range(4):
            fw.op("pe", lambda e, h=h: e.matmul(psT[h][:, :], lhsT=mkT[:, h, cc], rhs=mqT[:, h, cc], start=True, stop=True),
                  [mkT, mqT], [psT[h]])
            fw.op("pe", lambda e, h=h: e.transpose(out=pk[h][:, :], in_=mkT[:, h, cc], identity=cx.ident[:]),
                  [mkT, cx.ident], [pk[h]])
            fw.op("dve", lambda e, h=h: e.scalar_tensor_tensor(out=PT[h][:], in0=psT[h][:, :], scalar=es[:, h:h + 1],
                                                               in1=tri[:], op0=ALU.mult, op1=ALU.mult),
                  [psT[h], es, tri], [PT[h]])
            fw.op("dve", lambda e, h=h: e.tensor_scalar(out=k2[h][:], in0=pk[h][:, :], scalar1=wk[:, h:h + 1],
                                                        scalar2=None, op0=ALU.mult), [pk[h], wk], [k2[h]])
        for h in range(4):
            fw.op("pe", lambda e, h=h: e.matmul(pdC[h][:, :], lhsT=k2[h][:], rhs=mv[:, c, h, :], start=True, stop=True),
                  [k2[h], mv], [pdC[h]])
            fw.op("pe", lambda e, h=h: e.matmul(pnum[h][:, :], lhsT=PT[h][:], rhs=mv[:, c, h, :], start=True, stop=(c == 0)),
                  [PT[h], mv], [pnum[h]], inc=(c == 0))
            if c > 0:
                fw.op("pe", lambda e, h=h: e.matmul(pnum[h][:, :], lhsT=mqT[:, h, cc], rhs=Cb[c % 2][:, h, :],
                                                    start=False, stop=True), [mqT, Cb[c % 2]], [pnum[h]])
        if c < 15:
            for h in range(4):
                fw.op("dve", lambda e, h=h: e.scalar_tensor_tensor(out=Cf[:, h, :], in0=Cf[:, h, :], scalar=EB[:, h:h + 1],
                                                                   in1=pdC[h][:, :], op0=ALU.mult, op1=ALU.add),
                      [Cf, EB, pdC[h]], [Cf])
            fw.op("pool", lambda e: e.tensor_copy(out=Cb[(c + 1) % 2][:], in_=Cf[:]), [Cf], [Cb[(c + 1) % 2]])
        for h in range(4):
            fw.op("dve", lambda e, h=h: e.tensor_tensor(out=dn[:, h:h + 1], in0=pnum[h][:, 128:129], in1=ebt[:, h:h + 1],
                                                        op=ALU.mult), [pnum[h], ebt], [dn])
        fw.op("dve", lambda e: e.tensor_scalar(out=coef[:], in0=dn[:], scalar1=-1.0, scalar2=1.0, op0=ALU.mult, op1=ALU.max),
              [dn], [coef])
        fw.op("dve", lambda e: e.tensor_tensor(out=dn[:], in0=dn[:], in1=coef[:], op=ALU.max), [dn, coef], [dn])
        fw.op("dve", lambda e: e.reciprocal(out=dn[:], in_=dn[:]), [dn], [dn])
        fw.op("dve", lambda e: e.tensor_tensor(out=coef[:], in0=dn[:], in1=ebt[:], op=ALU.mult), [dn, ebt], [coef])
        for h in range(4):
            fw.op("act", lambda e, h=h: e.activation(out=hh[:, h, :], in_=pnum[h][:, 0:128], func=AF.Copy,
                                                     scale=coef[:, h:h + 1]), [pnum[h], coef], [hh])
        for h in range(4):
            fw.op("act", lambda e, h=h: e.activation(out=junk2[:], in_=hh[:, h, :], func=AF.Square,
                                                     accum_out=ssq4[:, h:h + 1]), [hh], [junk2, ssq4])
        fw.op("act", lambda e: e.activation(out=rstd4[:], in_=ssq4[:], func=AF.Ln, scale=1.0 / 128, bias=EPS), [ssq4], [rstd4])
        fw.op("act", lambda e: e.activation(out=rstd4[:], in_=rstd4[:], func=AF.Exp, scale=-0.5), [rstd4], [rstd4])
        fw.op("dve", lambda e: e.tensor_tensor(out=ymf[:].rearrange("p (h d) -> p h d", h=4), in0=hh[:],
                                               in1=rstd4[:].unsqueeze(2).broadcast_to([128, 4, 128]), op=ALU.mult),
              [hh, rstd4], [ymf])
        fw.op("dve", lambda e: e.tensor_tensor(out=ymf[:], in0=ymf[:], in1=mng[:], op=ALU.mult), [ymf, mng], [ymf])
        fw.op("dve", lambda e: e.tensor_tensor(out=ymb[:], in0=ymf[:], in1=sigo[:, c, :], op=ALU.mult), [ymf, sigo], [ymb])
        if dbg is not None and b == 0:
            fw.dma("sp", dbg["y_m"][c * 128:(c + 1) * 128, :], ymf[:], reads=[ymf], writes=[dbg["y_m"]])
        transpose_to(fw, cx, ymb, yTm, slice(0, 128), pyt, nblk=4)
        for hf in range(2):
            for kc in range(8):
                if kc < 4:
                    lt, lb = yT_nsa[:, kc, cc], yT_nsa
                else:
                    lt, lb = yTm[:, kc - 4, :], yTm
                fw.op("pe", lambda e, kc=kc, hf=hf, lt=lt: e.matmul(ph[hf][:], lhsT=lt, rhs=wout[:, kc, hf * 512:(hf + 1) * 512],
                                                                    start=(kc == 0), stop=(kc == 7)),
                      [lb, wout], [ph[hf]], inc=(kc == 7))
        post_residual(fw, cx, ph[0], ph[1], xres[i], gpost, 1.0, ssq2, rs2, junk, tmp)
        fw.dma("sp", dst[r0:r0 + 128, :], xres[i][:], reads=[xres[i]], writes=[dst])
    fw.release(m3)


W_NAMES = ["norm_g", "ffn1_w_gu", "ffn1_w_down", "ffn2_w_gu", "ffn2_w_down", "mix_w_in", "mix_w_out",
           "nsa_cmp_pe", "nsa_cmp_w1", "nsa_cmp_w2", "mlstm_conv_w", "mlstm_conv_b", "mlstm_i_bias",
           "mlstm_f_bias", "mlstm_norm_g"]
W_SHAPES = {
    "norm_g": [2, 6, 1024], "ffn1_w_gu": [2, 1024, 5632], "ffn1_w_down": [2, 2816, 1024],
    "ffn2_w_gu": [2, 1024, 5632], "ffn2_w_down": [2, 2816, 1024], "mix_w_in": [2, 1024, 3360],
    "mix_w_out": [2, 1024, 1024], "nsa_cmp_pe": [2, 2, 32, 64], "nsa_cmp_w1": [2, 2, 2048, 128],
    "nsa_cmp_w2": [2, 2, 128, 64], "mlstm_conv_w": [2, 4, 1024], "mlstm_conv_b": [2, 1024],
    "mlstm_i_bias": [2, 4], "mlstm_f_bias": [2, 4], "mlstm_norm_g": [2, 512],
}


def host_consts():
    c = {}
    c["c_ident"] = np.eye(128, dtype=np.float32)
    pos = np.arange(S, dtype=np.float32)
    inv = (500000.0 ** (-np.arange(0, 16, 2, dtype=np.float32) / 16.0)).astype(np.float32)
    ang = (pos[None, :] * inv[:, None]).astype(np.float32)
    cs, sn = np.cos(ang).astype(np.float32), np.sin(ang).astype(np.float32)
    c["c_rope_c"] = np.concatenate([cs, cs], 0)
    c["c_rope_s"] = np.concatenate([-sn, sn], 0)
    perm = np.zeros((64, 16), np.float32)
    for i in range(8):
        perm[i + 8, i] = 1.0
        perm[i, i + 8] = 1.0
    c["c_perm"] = perm
    key = np.arange(S)
    c["c_E"] = (key[None, :] // 64 == np.arange(32)[:, None]).astype(np.float32)
    kk, qq = np.arange(128)[:, None], np.arange(128)[None, :]
    c["c_causal"] = np.where(kk > qq, -BIG, 0.0).astype(np.float32)
    c["c_anti"] = np.where(kk <= qq, -BIG, 0.0).astype(np.float32)
    n = np.arange(128)[:, None]
    c["c_cmpneg"] = np.where(16 * n + 31 > key[None, :], -BIG, 0.0).astype(np.float32)
    ovl = np.zeros((128, 33), np.float32)
    ovl[:, 0] = 1.0
    ci = np.arange(127)[:, None] * 16
    sj = np.arange(32)[None, :] * 64
    ovl[:127, 1:] = ((ci < sj + 64) & (ci + 32 > sj)).astype(np.float32)
    c["c_ovl"] = ovl
    t = np.arange(S)[:, None]
    j = np.arange(32)[None, :]
    cur = t // 64
    forced = (j == 0) | (j == cur) | (j == cur - 1)
    invalid = j * 64 > t
    c["c_addm"] = np.where(forced | invalid, -1e30, 0.0).astype(np.float32)
    c["c_forced"] = forced.astype(np.float32)
    c["c_tri"] = (kk <= qq).astype(np.float32)
    return c


def build(phases=("f1", "mix", "f2"), depth=DEPTH, ntiles=T // 512, debug=False):
    nc = bass.Bass("TRN2", target_bir_lowering=False)
    fw = FW(nc)
    cx = Ctx()
    x_in = fw.dram("x", [T, D], F32, kind="ExternalInput")
    y_out = fw.dram("y", [T, D], F32, kind="ExternalOutput")
    W = {n: fw.dram(n, W_SHAPES[n], F32, kind="ExternalInput") for n in W_NAMES}
    C = {n: fw.dram(n, list(v.shape), F32, kind="ExternalInput") for n, v in host_consts().items()}
    scr = [fw.dram("scrA", [T, D], F32), fw.dram("scrB", [T, D], F32)]

    cx.ident = fw.sb([128, 128], BF16, "ident")
    fw.dma("pool", cx.ident[:], C["c_ident"][:, :], reads=[C["c_ident"]], writes=[cx.ident])
    cx.perm = fw.sb([64, 16], BF16, "perm")
    fw.dma("pool", cx.perm[:], C["c_perm"][:, :], reads=[C["c_perm"]], writes=[cx.perm])
    dbg = None
    if debug:
        dbg = {"yT_nsa": fw.dram("dbg_yT_nsa", [128, 4, S], F32, kind="ExternalOutput"),
               "y_m": fw.dram("dbg_y_m", [S, 512], F32, kind="ExternalOutput")}
    cx.eps_t = fw.sb([128, 1], F32, "eps")
    cx.eps4_t = fw.sb([128, 1], F32, "eps4")
    fw.op("dve", lambda e: e.memset(cx.eps_t[:], EPS), [], [cx.eps_t])
    fw.op("dve", lambda e: e.memset(cx.eps4_t[:], 4.0 * EPS), [], [cx.eps4_t])

    plan = []
    for l in range(depth):
        for p in phases:
            plan.append((l, p))
    cur = x_in
    for i, (l, p) in enumerate(plan):
        dst = y_out if i == len(plan) - 1 else scr[i % 2]
        if p == "f1":
            ffn_phase(fw, cx, cur, dst, W["ffn1_w_gu"], W["ffn1_w_down"], W["norm_g"], l, 0, 1, ntiles)
        elif p == "f2":
            ffn_phase(fw, cx, cur, dst, W["ffn2_w_gu"], W["ffn2_w_down"], W["norm_g"], l, 4, 5, ntiles)
        else:
            mixer_phase(fw, cx, cur, dst, W, C, l, dbg)
        cur = dst
    fw.finish()
    return nc


_CACHE = {}


def kernel(**inputs):
    x = np.ascontiguousarray(np.asarray(inputs["x"], dtype=np.float32))
    ncores = 8
    if "nc" not in _CACHE:
        _CACHE["nc"] = build()
    nc = _CACHE["nc"]
    consts = host_consts()
    shared = {n: np.ascontiguousarray(np.asarray(inputs[n], dtype=np.float32)) for n in W_NAMES}
    shared.update(consts)
    in_maps = []
    for c in range(ncores):
        m = dict(shared)
        m["x"] = x[c * NB:(c + 1) * NB].reshape(T, D)
        in_maps.append(m)
    res = run_bass_kernel_spmd(nc, in_maps, core_ids=list(range(ncores)))
    out = np.concatenate([r["y"].reshape(NB, S, D) for r in res.results], axis=0)
    return out.astype(np.float32)
```

```python
import math
import numpy as np
import ml_dtypes
import concourse.bass as bass
import concourse.mybir as mybir
from concourse.bass_utils import run_bass_kernel_spmd

F32 = mybir.dt.float32
BF16 = mybir.dt.bfloat16
AF = mybir.ActivationFunctionType
ALU = mybir.AluOpType

D = 1024
S = 2048
NB = 2
T = NB * S
DFF = 2816
NJ = DFF // 128
INC = 3360
EPS = 1e-6
BIG = 30000.0
N_CMP = 127
DEPTH = 2


def _dtsize(dt):
    return 4 if dt == F32 else 2


class Buf:
    __slots__ = ("t", "_w", "_r", "name", "root")

    def __init__(self, t, name="", root=None):
        self.t = t
        self._w = None
        self._r = {}
        self.name = name
        self.root = root

    @property
    def w(self):
        return self.root._w if self.root is not None else self._w

    @w.setter
    def w(self, v):
        if self.root is not None:
            self.root._w = v
        else:
            self._w = v

    @property
    def r(self):
        return self.root._r if self.root is not None else self._r

    @r.setter
    def r(self, v):
        if self.root is not None:
            self.root._r = v
        else:
            self._r = v

    def __getitem__(self, k):
        return self.t[k]


class FW:
    NDMA = 24

    def __init__(self, nc):
        self.nc = nc
        self.eng = {"pe": nc.tensor, "act": nc.scalar, "dve": nc.vector, "pool": nc.gpsimd, "sp": nc.sync}
        self.sem = {k: nc.alloc_semaphore("s_" + k) for k in self.eng}
        self.cnt = {k: 0 for k in self.eng}
        self.waited = {k: {} for k in self.eng}
        self.pending = {k: ([], []) for k in self.eng}
        self.dsem = [nc.alloc_semaphore("d%d" % i) for i in range(2 * self.NDMA)]
        self.dval = [0] * (2 * self.NDMA)
        self.dnext = {"sp": 0, "pool": 0}
        self.nbuf = 0
        self.sb_lo = 16512
        self.sb_hi = 229344
        self.sb_ptr = self.sb_lo
        self.banks = [nc.alloc_psum_tensor("bank%d" % i, [128, 512], F32) for i in range(8)]
        self.bank_root = [Buf(self.banks[i], "bankroot%d" % i) for i in range(8)]

    def sb(self, shape, dt, name=None):
        self.nbuf += 1
        name = (name or "sb") + "_%d" % self.nbuf
        nbytes = int(np.prod(shape[1:])) * _dtsize(dt)
        nbytes = (nbytes + 31) // 32 * 32
        off = self.sb_ptr
        self.sb_ptr += nbytes
        assert self.sb_ptr <= self.sb_hi, "SBUF overflow %s: %d > %d" % (name, self.sb_ptr, self.sb_hi)
        return Buf(self.nc.alloc_sbuf_tensor_at(name, list(shape), dt, offset=off), name)

    def mark(self):
        return self.sb_ptr

    def release(self, m):
        self.barrier()
        self.sb_ptr = m

    def bank(self, i, dt=F32, lo=0, hi=512, name=None):
        ap = self.banks[i][:, lo:hi]
        if dt != F32:
            ap = ap.bitcast(dt)
        return Buf(ap, name or ("bank%d_%d" % (i, lo)), root=self.bank_root[i])

    def dram(self, name, shape, dt, kind="Internal"):
        return Buf(self.nc.dram_tensor(name, list(shape), dt, kind=kind).ap(), name)

    def _wait(self, e, tok):
        if tok is None:
            return
        key, sem, val = tok
        cur = self.waited[e].get(key, 0)
        if cur >= val:
            return
        self.waited[e][key] = val
        self.eng[e].wait_ge(sem, val)

    def _deps(self, e, reads, writes):
        for b in reads:
            self._wait(e, b.w)
        for b in writes:
            self._wait(e, b.w)
            for t in b.r.values():
                self._wait(e, t)

    def _commit(self, tok, reads, writes):
        for b in reads:
            o = b.r.get(tok[0])
            if o is None or o[2] < tok[2]:
                b.r[tok[0]] = tok
        for b in writes:
            b.w = tok
            b.r = {}

    def op(self, e, ins_fn, reads=(), writes=(), inc=True):
        reads = [b for b in reads if b is not None]
        writes = [b for b in writes if b is not None]
        self._deps(e, reads, writes)
        ins = ins_fn(self.eng[e])
        if inc:
            self.cnt[e] += 1
            ins.then_inc(self.sem[e], 1)
            tok = (e, self.sem[e], self.cnt[e])
            pr, pw = self.pending[e]
            self._commit(tok, reads + pr, writes + pw)
            self.pending[e] = ([], [])
        else:
            self.pending[e][0].extend(reads)
            self.pending[e][1].extend(writes)
        return ins

    def dma(self, q, out, in_, reads=(), writes=(), **kw):
        reads = [b for b in reads if b is not None]
        writes = [b for b in writes if b is not None]
        self._deps(q, reads, writes)
        i = self.dnext[q] + (self.NDMA if q == "pool" else 0)
        self.dnext[q] = (self.dnext[q] + 1) % self.NDMA
        key = "d%d" % i
        if self.dval[i] > 0:
            self._wait(q, (key, self.dsem[i], self.dval[i]))
        self.dval[i] += 16
        self.eng[q].dma_start(out=out, in_=in_, **kw).then_inc(self.dsem[i], 16)
        tok = (key, self.dsem[i], self.dval[i])
        self._commit(tok, reads, writes)
        return tok

    def barrier(self):
        toks = [(k, self.sem[k], self.cnt[k]) for k in self.eng if self.cnt[k] > 0]
        toks += [("d%d" % i, self.dsem[i], self.dval[i]) for i in range(2 * self.NDMA) if self.dval[i] > 0]
        for e in self.eng:
            for t in toks:
                self._wait(e, t)

    def finish(self):
        self.barrier()


class Ctx:
    pass


def load_bcast(fw, dst, src_row_ap, src_buf, q="sp"):
    fw.dma(q, dst[:], src_row_ap.partition_broadcast(128), reads=[src_buf], writes=[dst])


def rms_prenorm(fw, cx, xt, gpre, xn, ssq, rs, junk):
    fw.op("act", lambda e: e.activation(out=junk[:], in_=xt[:], func=AF.Square, accum_out=ssq[:]),
          [xt], [junk, ssq])
    fw.op("act", lambda e: e.activation(out=rs[:], in_=ssq[:], func=AF.Sqrt, scale=1.0 / D, bias=EPS),
          [ssq], [rs])
    fw.op("dve", lambda e: e.reciprocal(out=rs[:], in_=rs[:]), [rs], [rs])
    fw.op("dve", lambda e: e.scalar_tensor_tensor(out=xn[:], in0=xt[:], scalar=rs[:, 0:1], in1=gpre[:],
                                                  op0=ALU.mult, op1=ALU.mult), [xt, rs, gpre], [xn])


def transpose_to(fw, cx, src, dstT, dst_cols, pst, nblk=8):
    for k in range(nblk):
        fw.op("pe", lambda e, k=k: e.transpose(out=pst[:, k * 128:(k + 1) * 128], in_=src[:, k * 128:(k + 1) * 128],
                                               identity=cx.ident[:]),
              [src, cx.ident], [pst], inc=(k == nblk - 1))
    fw.op("dve", lambda e: e.tensor_copy(out=dstT[:, 0:nblk, dst_cols],
                                         in_=pst[:, 0:nblk * 128].rearrange("p (k t) -> p k t", k=nblk)),
          [pst], [dstT])


def post_residual(fw, cx, ph0, ph1, xr, gpost, half_scale, ssq2, rs2, junk, tmp, lnexp=False, tmp2=None):
    fw.op("act", lambda e: e.activation(out=junk[:, 0:512], in_=ph0[:], func=AF.Square, accum_out=ssq2[:, 0:1]),
          [ph0], [junk, ssq2])
    fw.op("act", lambda e: e.activation(out=junk[:, 0:512], in_=ph1[:], func=AF.Square, accum_out=ssq2[:, 1:2]),
          [ph1], [junk, ssq2])
    fw.op("dve", lambda e: e.tensor_tensor(out=rs2[:], in0=ssq2[:, 0:1], in1=ssq2[:, 1:2], op=ALU.add),
          [ssq2], [rs2])
    if lnexp:
        fw.op("act", lambda e: e.activation(out=rs2[:], in_=rs2[:], func=AF.Ln, scale=1.0 / D, bias=EPS), [rs2], [rs2])
        fw.op("act", lambda e: e.activation(out=rs2[:], in_=rs2[:], func=AF.Exp, scale=-0.5, bias=math.log(half_scale)),
              [rs2], [rs2])
    else:
        k = 1.0 / (half_scale * half_scale)
        fw.op("act", lambda e: e.activation(out=rs2[:], in_=rs2[:], func=AF.Sqrt, scale=k / D, bias=k * EPS),
              [rs2], [rs2])
        fw.op("dve", lambda e: e.reciprocal(out=rs2[:], in_=rs2[:]), [rs2], [rs2])
    tmps = [tmp, tmp2 if tmp2 is not None else tmp]
    add_eng = "pool" if tmp2 is not None else "dve"
    for hf, ph in ((0, ph0), (1, ph1)):
        sl = slice(hf * 512, (hf + 1) * 512)
        t_ = tmps[hf]
        fw.op("dve", lambda e, ph=ph, sl=sl, t_=t_: e.scalar_tensor_tensor(out=t_[:], in0=ph[:], scalar=rs2[:, 0:1],
                                                                           in1=gpost[:, sl], op0=ALU.mult, op1=ALU.mult),
              [ph, rs2, gpost], [t_])
        fw.op(add_eng, lambda e, sl=sl, t_=t_: e.tensor_tensor(out=xr[:, sl], in0=xr[:, sl], in1=t_[:], op=ALU.add),
              [xr, t_], [xr])


def ffn_phase(fw, cx, src, dst, w_gu, w_dn, g_all, l, gi_pre, gi_post, ntiles=T // 512):
    nc = fw.nc
    m0 = fw.mark()
    wgu_t = fw.sb([128, 8, 2 * DFF], BF16, "wgu")
    wdn_t = fw.sb([128, NJ, D], BF16, "wdn")
    wgu_g = [Buf(wgu_t.t, "wgu_g%d" % j) for j in range(NJ)]
    wgu_u = [Buf(wgu_t.t, "wgu_u%d" % j) for j in range(NJ)]
    wdn_b = [Buf(wdn_t.t, "wdn%d" % j) for j in range(NJ)]
    gpre = fw.sb([128, D], F32, "gpre")
    gpost = fw.sb([128, D], F32, "gpost")
    xnT = fw.sb([128, 8, 512], BF16, "xnT")
    actT = fw.sb([128, NJ, 512], BF16, "actT")
    actT_j = [Buf(actT.t, "actT%d" % j) for j in range(NJ)]
    xin = [fw.sb([128, D], F32, "xin") for _ in range(2)]
    xres = [fw.sb([128, D], F32, "xres") for _ in range(2)]
    xn = [fw.sb([128, D], BF16, "xn") for _ in range(2)]
    sg = [fw.sb([128, 512], BF16, "sg") for _ in range(2)]
    tmp = [fw.sb([128, 512], F32, "tmp") for _ in range(2)]
    junk = fw.sb([128, D], BF16, "junk")
    ssq = [fw.sb([128, 1], F32, "ssq") for _ in range(2)]
    rs = [fw.sb([128, 1], F32, "rs") for _ in range(2)]
    ssq2 = [fw.sb([128, 2], F32, "ssq2") for _ in range(2)]
    rs2 = [fw.sb([128, 1], F32, "rs2") for _ in range(2)]
    pg = [fw.bank(0), fw.bank(1)]
    pu = [fw.bank(2), fw.bank(3)]
    po = [[fw.bank(4), fw.bank(5)], [fw.bank(6), fw.bank(7)]]
    pst = [fw.bank(6, BF16), fw.bank(7, BF16)]

    load_bcast(fw, gpre, g_all[l, gi_pre:gi_pre + 1, :], g_all)
    load_bcast(fw, gpost, g_all[l, gi_post:gi_post + 1, :], g_all)
    wsrc = w_gu[l].rearrange("(kc p) n -> p kc n", p=128)
    for j in range(NJ):
        fw.dma("pool", wgu_t[:, :, j * 128:(j + 1) * 128], wsrc[:, :, j * 128:(j + 1) * 128],
               reads=[w_gu], writes=[wgu_g[j]])
        fw.dma("pool", wgu_t[:, :, DFF + j * 128:DFF + (j + 1) * 128], wsrc[:, :, DFF + j * 128:DFF + (j + 1) * 128],
               reads=[w_gu], writes=[wgu_u[j]])
    for j in range(NJ):
        fw.dma("pool", wdn_t[:, j, :], w_dn[l, j * 128:(j + 1) * 128, :], reads=[w_dn], writes=[wdn_b[j]])

    cnt = [0]

    def pre(tile):
        for s in range(4):
            i = cnt[0] % 2
            cnt[0] += 1
            r0 = tile * 512 + s * 128
            fw.dma("sp", xin[i][:], src[r0:r0 + 128, :], reads=[src], writes=[xin[i]])
            rms_prenorm(fw, cx, xin[i], gpre, xn[i], ssq[i], rs[i], junk)
            transpose_to(fw, cx, xn[i], xnT, slice(s * 128, (s + 1) * 128), pst[i])

    pre(0)
    for tile in range(ntiles):
        for j in range(NJ):
            a, b = pg[j % 2], pu[j % 2]
            for kc in range(8):
                fw.op("pe", lambda e, kc=kc, a=a: e.matmul(a[:], lhsT=wgu_t[:, kc, j * 128:(j + 1) * 128],
                                                           rhs=xnT[:, kc, :], start=(kc == 0), stop=(kc == 7)),
                      [wgu_g[j], xnT], [a], inc=(kc == 7))
            for kc in range(8):
                fw.op("pe", lambda e, kc=kc, b=b: e.matmul(b[:], lhsT=wgu_t[:, kc, DFF + j * 128:DFF + (j + 1) * 128],
                                                           rhs=xnT[:, kc, :], start=(kc == 0), stop=(kc == 7)),
                      [wgu_u[j], xnT], [b], inc=(kc == 7))
            s_ = sg[j % 2]
            fw.op("act", lambda e, a=a, s_=s_: e.activation(out=s_[:], in_=a[:], func=AF.Silu), [a], [s_])
            fw.op("dve", lambda e, b=b, s_=s_: e.tensor_tensor(out=actT[:, j, :], in0=b[:], in1=s_[:], op=ALU.mult),
                  [b, s_], [actT_j[j]])
        if tile + 1 < ntiles:
            pre(tile + 1)
        for s in range(4):
            i = s % 2
            r0 = tile * 512 + s * 128
            fw.dma("sp", xres[i][:], src[r0:r0 + 128, :], reads=[src], writes=[xres[i]])
            for hf in range(2):
                for j in range(NJ):
                    fw.op("pe", lambda e, j=j, hf=hf: e.matmul(po[i][hf][:], lhsT=actT[:, j, s * 128:(s + 1) * 128],
                                                               rhs=wdn_t[:, j, hf * 512:(hf + 1) * 512],
                                                               start=(j == 0), stop=(j == NJ - 1)),
                          [actT_j[j], wdn_b[j]], [po[i][hf]], inc=(j == NJ - 1))
            post_residual(fw, cx, po[i][0], po[i][1], xres[i], gpost, 0.5, ssq2[i], rs2[i], junk, tmp[i])
            fw.dma("sp", dst[r0:r0 + 128, :], xres[i][:], reads=[xres[i]], writes=[dst])
    fw.release(m0)


C_Q, C_KCMP, C_VCMP, C_KSLC, C_VSLC, C_KWIN, C_VWIN, C_GATE = 0, 512, 640, 768, 896, 1024, 1152, 1280
C_MQ, C_MK, C_MV, C_MO, C_MI, C_MF = 1304, 1816, 2328, 2840, 3352, 3356
SCALE = 0.125
MSCALE = 128.0 ** -0.5


def make_uT(fw, cx, src, b, gpre, uT, xin, xn, ssq, rs, junk, pst):
    for s in range(16):
        i = s % 2
        r0 = b * S + s * 128
        fw.dma("sp", xin[i][:], src[r0:r0 + 128, :], reads=[src], writes=[xin[i]])
        rms_prenorm(fw, cx, xin[i], gpre, xn[i], ssq[i], rs[i], junk)
        transpose_to(fw, cx, xn[i], uT[s // 4], slice(s * 128, (s + 1) * 128), pst[i])


class WStream:
    def __init__(self, fw, w_in, l, nbuf=2):
        self.fw = fw
        self.w_in = w_in
        self.src = w_in[l].rearrange("(kc p) n -> p kc n", p=128)
        self.bufs = [fw.sb([128, 8, 512], BF16, "wbuf") for _ in range(nbuf)]
        self.i = 0

    def load(self, c0, c1):
        wb = self.bufs[self.i % len(self.bufs)]
        self.i += 1
        self.fw.dma("pool", wb[:, :, 0:c1 - c0], self.src[:, :, c0:c1], reads=[self.w_in], writes=[wb])
        return wb


def proj_fm(fw, wb, off, M, uT, tile, ps):
    for kc in range(8):
        fw.op("pe", lambda e, kc=kc: e.matmul(ps[0:M, :], lhsT=wb[:, kc, off:off + M],
                                              rhs=uT[tile][:, kc, tile * 512:(tile + 1) * 512],
                                              start=(kc == 0), stop=(kc == 7)),
              [wb, uT[tile]], [ps], inc=(kc == 7))


def proj_tm(fw, wb, off, N, uT, s, ps):
    for kc in range(8):
        fw.op("pe", lambda e, kc=kc: e.matmul(ps[:, 0:N], lhsT=uT[s // 4][:, kc, s * 128:(s + 1) * 128],
                                              rhs=wb[:, kc, off:off + N], start=(kc == 0), stop=(kc == 7)),
              [wb, uT[s // 4]], [ps], inc=(kc == 7))


def rope_inplace(fw, cx, dst_buf, dst_ap64, dst_ap16, pq, tile, psw, t1, t2):
    cols = slice(tile * 512, (tile + 1) * 512)
    fw.op("pe", lambda e: e.matmul(psw[0:16, :], lhsT=cx.perm[0:64, 0:16], rhs=dst_ap64, start=True, stop=True),
          [cx.perm, dst_buf], [psw])
    fw.op("dve", lambda e: e.tensor_tensor(out=t1[:], in0=psw[0:16, :], in1=cx.ropeS[:, cols], op=ALU.mult),
          [psw, cx.ropeS], [t1])
    fw.op("dve", lambda e: e.tensor_tensor(out=t2[:], in0=pq[0:16, :], in1=cx.ropeC[:, cols], op=ALU.mult),
          [pq, cx.ropeC], [t2])
    fw.op("dve", lambda e: e.tensor_tensor(out=dst_ap16, in0=t1[:], in1=t2[:], op=ALU.add),
          [t1, t2], [dst_buf])


def mixer_phase(fw, cx, src, dst, W, C, l, dbg=None):
    nc = fw.nc
    w_in = W["mix_w_in"]
    mP = fw.mark()
    gpre = fw.sb([128, D], F32, "gpre")
    gpost = fw.sb([128, D], F32, "gpost")
    load_bcast(fw, gpre, W["norm_g"][l, 2:3, :], W["norm_g"])
    load_bcast(fw, gpost, W["norm_g"][l, 3:4, :], W["norm_g"])
    ibfb = fw.sb([128, 8], F32, "ibfb")
    fw.dma("sp", ibfb[:, 0:4], W["mlstm_i_bias"][l:l + 1, :].partition_broadcast(128), reads=[W["mlstm_i_bias"]], writes=[ibfb])
    fw.dma("sp", ibfb[:, 4:8], W["mlstm_f_bias"][l:l + 1, :].partition_broadcast(128), reads=[W["mlstm_f_bias"]], writes=[ibfb])
    mng = fw.sb([128, 512], F32, "mng")
    load_bcast(fw, mng, W["mlstm_norm_g"][l:l + 1, :], W["mlstm_norm_g"])
    cw = fw.sb([128, 8, 4], F32, "cw")
    cb = fw.sb([128, 8], F32, "cb")
    for c in range(8):
        fw.dma("sp", cw[:, c, :], W["mlstm_conv_w"][l, :, c * 128:(c + 1) * 128].rearrange("j p -> p j"),
               reads=[W["mlstm_conv_w"]], writes=[cw], allow_slow_non_contiguous=True)
        fw.dma("sp", cb[:, c:c + 1], W["mlstm_conv_b"][l:l + 1, c * 128:(c + 1) * 128].rearrange("o p -> p o"),
               reads=[W["mlstm_conv_b"]], writes=[cb], allow_slow_non_contiguous=True)
    w2sb = fw.sb([128, 2, 64], BF16, "w2sb")
    fw.dma("pool", w2sb[:], W["nsa_cmp_w2"][l].rearrange("k h d -> h k d"), reads=[W["nsa_cmp_w2"]], writes=[w2sb])
    cbias = fw.sb([128, 2], F32, "cbias")
    mb = fw.mark()
    w1r = fw.sb([128, 2, 16, 128], BF16, "w1r")
    pef = fw.sb([128, 2, 16], BF16, "pef")
    for kv in range(2):
        fw.dma("pool", w1r[:, kv], W["nsa_cmp_w1"][l, kv].rearrange("(p c) h -> p c h", c=16),
               reads=[W["nsa_cmp_w1"]], writes=[w1r])
        fw.dma("pool", pef[:, kv], W["nsa_cmp_pe"][l, kv].rearrange("l d -> (l d)").rearrange("(p c) -> p c", c=16),
               reads=[W["nsa_cmp_pe"]], writes=[pef])
    pb = fw.bank(0)
    for kv in range(2):
        for c in range(16):
            fw.op("pe", lambda e, kv=kv, c=c: e.matmul(pb[:, kv:kv + 1], lhsT=w1r[:, kv, c, :], rhs=pef[:, kv, c:c + 1],
                                                       start=(c == 0), stop=(c == 15)),
                  [w1r, pef], [pb], inc=(c == 15))
    fw.op("dve", lambda e: e.tensor_copy(out=cbias[:], in_=pb[:, 0:2]), [pb], [cbias])
    fw.release(mb)

    for b in range(NB):
        mB = fw.mark()
        yT_nsa = fw.sb([128, 4, S], BF16, "yTnsa")
        uT_ = fw.sb([128, 8, S], BF16, "uT")
        uT = [Buf(uT_.t, "uT%d" % i) for i in range(4)]
        m1 = fw.mark()
        qaug = [fw.sb([96, 16, 4, 128], BF16, "qaug") for _ in range(2)]
        kslc = fw.sb([96, 2, S], BF16, "kslc")
        kwin = fw.sb([64, 2, S], BF16, "kwin")
        vslc = fw.sb([128, 16, 2, 65], BF16, "vslc")
        vwin = fw.sb([128, 16, 2, 65], BF16, "vwin")
        sig = fw.sb([128, 16, 24], F32, "sig")
        kcT = fw.sb([64, 2, 128], BF16, "kcT")
        vcx = fw.sb([128, 2, 97], BF16, "vcx")
        m1b = fw.mark()
        ws = WStream(fw, w_in, l)
        kvcmp = fw.sb([128, 2, S], BF16, "kvcmp")
        w1sb = fw.sb([128, 2, 32, 128], BF16, "w1sb")
        ropeC = fw.sb([16, S], F32, "ropeC")
        ropeS = fw.sb([16, S], F32, "ropeS")
        cx.ropeC, cx.ropeS = ropeC, ropeS
        xin = [fw.sb([128, D], F32, "xin") for _ in range(2)]
        xn = [fw.sb([128, D], BF16, "xn") for _ in range(2)]
        junk = fw.sb([128, D], BF16, "junk")
        ssq = [fw.sb([128, 1], F32, "ssq") for _ in range(2)]
        rs = [fw.sb([128, 1], F32, "rs") for _ in range(2)]
        t1 = [fw.sb([16, 512], F32, "t1") for _ in range(3)]
        t2 = [fw.sb([16, 512], F32, "t2") for _ in range(3)]
        gel = [fw.sb([128, 128], BF16, "gel") for _ in range(2)]
        fw.dma("sp", ropeC[:], C["c_rope_c"][:, :], reads=[C["c_rope_c"]], writes=[ropeC])
        fw.dma("sp", ropeS[:], C["c_rope_s"][:, :], reads=[C["c_rope_s"]], writes=[ropeS])
        for kv in range(2):
            for g in range(2):
                fw.dma("pool", w1sb[g * 64:(g + 1) * 64, kv], W["nsa_cmp_w1"][l, kv].rearrange("(l d) h -> d l h", d=64),
                       reads=[W["nsa_cmp_w1"]], writes=[w1sb])
        fw.dma("pool", kslc[64:96, 0, :], C["c_E"][:, :], reads=[C["c_E"]], writes=[kslc])
        fw.dma("pool", kslc[64:96, 1, :], C["c_E"][:, :], reads=[C["c_E"]], writes=[kslc])
        for g in range(2):
            fw.dma("pool", vcx[:, g, 64:97], C["c_ovl"][:, :], reads=[C["c_ovl"]], writes=[vcx])
            fw.op("pool", lambda e, g=g: e.memset(qaug[g][64:96], 0.0), [], [qaug[g]])
        fw.op("pool", lambda e: e.memset(vslc[:, :, :, 64:65], 1.0), [], [vslc])
        fw.op("pool", lambda e: e.memset(vwin[:, :, :, 64:65], 1.0), [], [vwin])
        pst = [fw.bank(6, BF16), fw.bank(7, BF16)]
        wb_q = ws.load(C_Q, C_Q + 512)
        wb_g1 = ws.load(C_KCMP, C_KCMP + 512)
        make_uT(fw, cx, src, b, gpre, uT, xin, xn, ssq, rs, junk, pst)
        pq = [fw.bank(0), fw.bank(1), fw.bank(2), fw.bank(3)]
        psw = [fw.bank(4), fw.bank(5)]
        ptm = [fw.bank(6), fw.bank(7)]
        k = 0
        wb = wb_q
        for h in range(8):
            g, hh = h // 4, h % 4
            for tile in range(4):
                p = pq[k % 4]
                proj_fm(fw, wb, h * 64, 64, uT, tile, p)
                d64 = qaug[g][0:64, tile * 4:(tile + 1) * 4, hh, :]
                d16 = qaug[g][0:16, tile * 4:(tile + 1) * 4, hh, :]
                fw.op("act", lambda e, p=p, d64=d64: e.activation(out=d64, in_=p[0:64, :].rearrange("p (a q) -> p a q", a=4),
                                                                  func=AF.Copy), [p], [qaug[g]])
                _rope(fw, cx, qaug[g], d64, d16, p, tile, psw[k % 2], t1[k % 3], t2[k % 3], four=True)
                k += 1
        wb_g2 = ws.load(C_KWIN, C_KWIN + 280)
        wb = wb_g1
        for tile in range(4):
            cols = slice(tile * 512, (tile + 1) * 512)
            for kv in range(2):
                p = pq[k % 4]
                proj_fm(fw, wb, kv * 128, 128, uT, tile, p)
                fw.op("act", lambda e, p=p, kv=kv: e.activation(out=kvcmp[:, kv, cols], in_=p[:, :], func=AF.Copy),
                      [p], [kvcmp])
                k += 1
            for g in range(2):
                p = pq[k % 4]
                proj_fm(fw, wb, 256 + g * 64, 64, uT, tile, p)
                d64 = kslc[0:64, g, cols]
                d16 = kslc[0:16, g, cols]
                fw.op("act", lambda e, p=p, d64=d64: e.activation(out=d64, in_=p[0:64, :], func=AF.Copy), [p], [kslc])
                _rope(fw, cx, kslc, d64, d16, p, tile, psw[k % 2], t1[k % 3], t2[k % 3])
                k += 1
        for s in range(16):
            p = ptm[s % 2]
            proj_tm(fw, wb, 384, 128, uT, s, p)
            fw.op("act", lambda e, p=p, s=s: e.activation(out=vslc[:, s, :, 0:64],
                                                          in_=p[:, 0:128].rearrange("p (g d) -> p g d", g=2), func=AF.Copy),
                  [p], [vslc])
        wb = wb_g2
        for tile in range(4):
            cols = slice(tile * 512, (tile + 1) * 512)
            for g in range(2):
                p = pq[k % 4]
                proj_fm(fw, wb, g * 64, 64, uT, tile, p)
                d64 = kwin[0:64, g, cols]
                d16 = kwin[0:16, g, cols]
                fw.op("act", lambda e, p=p, d64=d64: e.activation(out=d64, in_=p[0:64, :], func=AF.Copy), [p], [kwin])
                _rope(fw, cx, kwin, d64, d16, p, tile, psw[k % 2], t1[k % 3], t2[k % 3])
                k += 1
        for s in range(16):
            p = ptm[s % 2]
            proj_tm(fw, wb, 128, 152, uT, s, p)
            fw.op("act", lambda e, p=p, s=s: e.activation(out=vwin[:, s, :, 0:64],
                                                          in_=p[:, 0:128].rearrange("p (g d) -> p g d", g=2), func=AF.Copy),
                  [p], [vwin])
            fw.op("act", lambda e, p=p, s=s: e.activation(out=sig[:, s, :], in_=p[:, 128:152], func=AF.Sigmoid),
                  [p], [sig])
        for kv in range(2):
            for g in range(2):
                p = pq[k % 4]
                gl = gel[k % 2]
                for li in range(32):
                    fw.op("pe", lambda e, li=li, p=p: e.matmul(p[:, 0:127], lhsT=w1sb[g * 64:(g + 1) * 64, kv, li, :],
                                                               rhs=kvcmp[g * 64:(g + 1) * 64, kv, li:li + 2017:16],
                                                               start=(li == 0), stop=(li == 31)),
                          [w1sb, kvcmp], [p], inc=(li == 31))
                fw.op("act", lambda e, p=p, gl=gl: e.activation(out=gl[:, 0:127], in_=p[:, 0:127], func=AF.Gelu_apprx_tanh,
                                                                bias=cbias[:, kv:kv + 1]), [p, cbias], [gl])
                p2 = psw[k % 2]
                if kv == 0:
                    fw.op("pe", lambda e, p2=p2, gl=gl: e.matmul(p2[0:64, 0:127], lhsT=w2sb[:, 0, :], rhs=gl[:, 0:127],
                                                                 start=True, stop=True), [w2sb, gl], [p2])
                    fw.op("dve", lambda e, p2=p2: e.tensor_copy(out=kcT[0:64, g, 0:127], in_=p2[0:64, 0:127]), [p2], [kcT])
                else:
                    fw.op("pe", lambda e, p2=p2, gl=gl: e.matmul(p2[0:127, 0:64], lhsT=gl[:, 0:127], rhs=w2sb[:, 1, :],
                                                                 start=True, stop=True), [w2sb, gl], [p2])
                    fw.op("dve", lambda e, p2=p2: e.tensor_copy(out=vcx[0:127, g, 0:64], in_=p2[0:127, 0:64]), [p2], [vcx])
                k += 1
        fw.release(m1b)
        nsa_attention(fw, cx, C, qaug, kslc, kwin, vslc, vwin, sig, kcT, vcx, yT_nsa)
        if dbg is not None and b == 0:
            tcp = fw.sb([128, 4, S], F32, "dbgcp")
            fw.op("dve", lambda e: e.tensor_copy(out=tcp[:], in_=yT_nsa[:]), [yT_nsa], [tcp])
            fw.dma("sp", dbg["yT_nsa"][:, :, :], tcp[:], reads=[tcp], writes=[dbg["yT_nsa"]])
        fw.release(m1)
        mlstm_and_out(fw, cx, src, dst, W, C, l, b, gpre, gpost, ibfb, mng, cw, cb, yT_nsa, dbg, uT)
        fw.release(mB)
    fw.release(mP)


def _rope(fw, cx, dst_buf, d64, d16, pq, tile, psw, t1, t2, four=False):
    cols = slice(tile * 512, (tile + 1) * 512)
    fw.op("pe", lambda e: e.matmul(psw[0:16, :].rearrange("p (a q) -> p a q", a=4) if four else psw[0:16, :],
                                   lhsT=cx.perm[0:64, 0:16], rhs=d64, start=True, stop=True),
          [cx.perm, dst_buf], [psw])
    fw.op("dve", lambda e: e.tensor_tensor(out=t1[:], in0=psw[0:16, :], in1=cx.ropeS[:, cols], op=ALU.mult),
          [psw, cx.ropeS], [t1])
    fw.op("dve", lambda e: e.tensor_tensor(out=t2[:], in0=pq[0:16, :], in1=cx.ropeC[:, cols], op=ALU.mult),
          [pq, cx.ropeC], [t2])
    o = d16
    a, b_ = (t1[:].rearrange("p (a q) -> p a q", a=4), t2[:].rearrange("p (a q) -> p a q", a=4)) if four else (t1[:], t2[:])
    fw.op("dve", lambda e: e.tensor_tensor(out=o, in0=a, in1=b_, op=ALU.add), [t1, t2], [dst_buf])


def nsa_attention(fw, cx, C, qaug, kslc, kwin, vslc, vwin, sig, kcT, vcx, yT_nsa):
    m = fw.mark()
    causal = fw.sb([128, 128], BF16, "causal")
    anti = fw.sb([128, 128], BF16, "anti")
    cmpneg = fw.sb([128, S], BF16, "cmpneg")
    addm = fw.sb([128, 16, 32], F32, "addm")
    forced = fw.sb([128, 16, 32], F32, "forced")
    fw.dma("pool", causal[:], C["c_causal"][:, :], reads=[C["c_causal"]], writes=[causal])
    fw.dma("pool", anti[:], C["c_anti"][:, :], reads=[C["c_anti"]], writes=[anti])
    fw.dma("pool", cmpneg[:], C["c_cmpneg"][:, :], reads=[C["c_cmpneg"]], writes=[cmpneg])
    fw.dma("sp", addm[:], C["c_addm"].t.rearrange("(c p) j -> p c j", p=128), reads=[C["c_addm"]], writes=[addm])
    fw.dma("sp", forced[:], C["c_forced"].t.rearrange("(c p) j -> p c j", p=128), reads=[C["c_forced"]], writes=[forced])
    pT = [fw.sb([128, 512], BF16, "pT") for _ in range(4)]
    l4 = [fw.sb([128, 4], F32, "l4") for _ in range(3)]
    cf = [fw.sb([128, 4], F32, "cf") for _ in range(3)]
    imp = fw.sb([128, 32], F32, "imp")
    impn = fw.sb([128, 32], F32, "impn")
    imp2 = fw.sb([128, 32], F32, "imp2")
    m8 = fw.sb([128, 16], F32, "m8")
    selb = fw.sb([128, 32], F32, "selb")
    negsel = fw.sb([128, 32], BF16, "negsel")
    yacc = fw.sb([128, 4, 64], F32, "yacc")
    ytmp = fw.sb([128, 4, 64], F32, "ytmp")
    yb = [fw.sb([128, 512], BF16, "yb") for _ in range(2)]
    psc = [fw.bank(0), fw.bank(1), fw.bank(2), fw.bank(3)]
    po_c = fw.bank(4, F32, 0, 388)
    po_s = fw.bank(5, F32, 0, 260)
    po_w = fw.bank(6, F32, 0, 260)
    ptr = fw.bank(7, BF16, 0, 64)
    pyt = fw.bank(7, BF16, 256, 512)
    po_c3 = po_c.t.rearrange("p (h c) -> p h c", h=4)
    po_s3 = po_s.t.rearrange("p (h c) -> p h c", h=4)
    po_w3 = po_w.t.rearrange("p (h c) -> p h c", h=4)
    sci = [0]

    def scores(lhsT_ap, lhs_buf, K, M, g, qt, mask_buf, mask_ap):
        i = sci[0] % 4
        sci[0] += 1
        ps, pt = psc[i], pT[i]
        ps3 = ps[0:M, :].rearrange("p (h q) -> p h q", h=4)
        fw.op("pe", lambda e: e.matmul(ps3, lhsT=lhsT_ap, rhs=qaug[g][0:K, qt], start=True, stop=(mask_buf is None)),
              [lhs_buf, qaug[g]], [ps], inc=(mask_buf is None))
        if mask_buf is not None:
            fw.op("pe", lambda e: e.matmul(ps3, lhsT=cx.ident[0:M, 0:M],
                                           rhs=mask_ap.unsqueeze(1).broadcast_to([M, 4, 128]), start=False, stop=True),
                  [cx.ident, mask_buf], [ps])
        return ps, pt

    def expo(ps, pt, M):
        fw.op("act", lambda e: e.activation(out=pt[0:M, :], in_=ps[0:M, :], func=AF.Exp, scale=SCALE), [ps], [pt])

    def pv(pt, M, po_buf, po3, v_ap, v_buf, first, last):
        for hh in range(4):
            fw.op("pe", lambda e, hh=hh: e.matmul(po3[:, hh, :], lhsT=pt[0:M, hh * 128:(hh + 1) * 128], rhs=v_ap,
                                                  start=(first and hh == 0), stop=(last and hh == 3),
                                                  skip_group_check=True),
                  [pt, v_buf], [po_buf], inc=(hh == 3))

    def coefs(br, po_buf, po3, g, qt):
        fw.op("dve", lambda e: e.tensor_scalar(out=l4[br][:], in0=po3[:, :, 64], scalar1=1e-30, scalar2=None, op0=ALU.max),
              [po_buf], [l4[br]])
        fw.op("dve", lambda e: e.reciprocal(out=l4[br][:], in_=l4[br][:]), [l4[br]], [l4[br]])
        c0 = br * 8 + g * 4
        fw.op("dve", lambda e: e.tensor_tensor(out=cf[br][:], in0=l4[br][:], in1=sig[:, qt, c0:c0 + 4], op=ALU.mult),
              [l4[br], sig], [cf[br]])

    yaccs = [yacc, fw.sb([128, 4, 64], F32, "yacc_b")]

    def after_cmp(ui, g, qt):
        ya = yaccs[ui % 2]
        coefs(0, po_c, po_c3, g, qt)
        if qt >= 8:
            fw.op("dve", lambda e: e.tensor_scalar(out=imp[:], in0=po_c3[:, 0, 65:97], scalar1=l4[0][:, 0:1],
                                                   scalar2=None, op0=ALU.mult), [po_c, l4[0]], [imp])
            for hh in range(1, 4):
                fw.op("dve", lambda e, hh=hh: e.scalar_tensor_tensor(out=imp[:], in0=po_c3[:, hh, 65:97],
                                                                     scalar=l4[0][:, hh:hh + 1], in1=imp[:],
                                                                     op0=ALU.mult, op1=ALU.add),
                      [po_c, l4[0], imp], [imp])
            fw.op("dve", lambda e: e.tensor_tensor(out=impn[:], in0=imp[:], in1=addm[:, qt, :], op=ALU.add),
                  [imp, addm], [impn])
            fw.op("dve", lambda e: e.max(out=m8[:, 0:8], in_=impn[:]), [impn], [m8])
            fw.op("dve", lambda e: e.match_replace(out=imp2[:], in_to_replace=m8[:, 0:8], in_values=impn[:],
                                                   imm_value=-1e30), [impn, m8], [imp2])
            fw.op("dve", lambda e: e.max(out=m8[:, 8:16], in_=imp2[:]), [imp2], [m8])
            fw.op("dve", lambda e: e.tensor_scalar(out=selb[:], in0=impn[:], scalar1=m8[:, 12:13], scalar2=None,
                                                   op0=ALU.is_ge), [impn, m8], [selb])
            fw.op("dve", lambda e: e.tensor_tensor(out=selb[:], in0=selb[:], in1=forced[:, qt, :], op=ALU.max),
                  [selb, forced], [selb])
            fw.op("dve", lambda e: e.tensor_scalar(out=negsel[:], in0=selb[:], scalar1=-1.0, scalar2=BIG,
                                                   op0=ALU.add, op1=ALU.mult), [selb], [negsel])
        fw.op("dve", lambda e: e.tensor_tensor(out=ya[:], in0=po_c3[:, :, 0:64],
                                               in1=cf[0][:].unsqueeze(2).broadcast_to([128, 4, 64]), op=ALU.mult),
              [po_c, cf[0]], [ya])

    def mask_to_q(g, qt):
        fw.op("pe", lambda e: e.transpose(out=ptr[0:32, 0:128], in_=negsel[:, :], identity=cx.ident[:]),
              [negsel, cx.ident], [ptr])
        fw.op("dve", lambda e: e.tensor_copy(out=qaug[g][64:96, qt],
                                             in_=ptr[0:32, 0:128].unsqueeze(1).broadcast_to([32, 4, 128])),
              [ptr], [qaug[g]])

    def after_win(ui, g, qt):
        ya = yaccs[ui % 2]
        coefs(2, po_w, po_w3, g, qt)
        fw.op("dve", lambda e: e.tensor_tensor(out=ytmp[:], in0=po_w3[:, :, 0:64],
                                               in1=cf[2][:].unsqueeze(2).broadcast_to([128, 4, 64]), op=ALU.mult),
              [po_w, cf[2]], [ytmp])
        fw.op("dve", lambda e: e.tensor_tensor(out=ya[:], in0=ya[:], in1=ytmp[:], op=ALU.add), [ya, ytmp], [ya])

    deferred = []

    def after_sel(ui, g, qt, at):
        ya = yaccs[ui % 2]
        ybq = yb[qt % 2]
        coefs(1, po_s, po_s3, g, qt)
        fw.op("dve", lambda e: e.tensor_tensor(out=ytmp[:], in0=po_s3[:, :, 0:64],
                                               in1=cf[1][:].unsqueeze(2).broadcast_to([128, 4, 64]), op=ALU.mult),
              [po_s, cf[1]], [ytmp])
        fw.op("dve", lambda e: e.tensor_tensor(out=ybq[:, g * 256:(g + 1) * 256].rearrange("p (h d) -> p h d", h=4),
                                               in0=ya[:], in1=ytmp[:], op=ALU.add), [ya, ytmp], [ybq])
        if g == 1:
            deferred.append((at + 3, lambda: transpose_to(fw, cx, ybq, yT_nsa, slice(qt * 128, (qt + 1) * 128), pyt, nblk=4)))

    units = [(qt, g) for qt in range(16) for g in range(2)]

    def cjob(ui):
        qt, g = units[ui]
        return ((kcT[0:64, g, 0:127], kcT, 64, 127, g, qt, cmpneg, cmpneg[0:127, qt * 128:(qt + 1) * 128]),
                (127, po_c, po_c3, vcx[0:127, g, :], vcx, True, True),
                (lambda at: after_cmp(ui, g, qt)), None)

    def wjobs(ui):
        qt, g = units[ui]
        out = []
        k0 = max(0, qt - 4)
        for kc in range(k0, qt + 1):
            ksl = slice(kc * 128, (kc + 1) * 128)
            if kc == qt:
                mb, ma = causal, causal[:, :]
            elif kc == qt - 4:
                mb, ma = anti, anti[:, :]
            else:
                mb, ma = None, None
            out.append(((kwin[0:64, g, ksl], kwin, 64, 128, g, qt, mb, ma),
                        (128, po_w, po_w3, vwin[:, kc, g, :], vwin, kc == k0, kc == qt),
                        (lambda at: after_win(ui, g, qt)) if kc == qt else None, None))
        return out

    def sjobs(ui):
        qt, g = units[ui]
        out = []
        for kc in range(qt + 1):
            ksl = slice(kc * 128, (kc + 1) * 128)
            out.append(((kslc[0:96, g, ksl], kslc, 96, 128, g, qt,
                         causal if kc == qt else None, causal[:, :] if kc == qt else None),
                        (128, po_s, po_s3, vslc[:, kc, g, :], vslc, kc == 0, kc == qt),
                        (lambda at: after_sel(ui, g, qt, at)) if kc == qt else None,
                        (lambda: mask_to_q(g, qt)) if (kc == 0 and qt >= 8) else None))
        return out

    jobs = [cjob(0)]
    for ui in range(len(units)):
        jobs += wjobs(ui)
        if ui + 1 < len(units):
            jobs.append(cjob(ui + 1))
        jobs += sjobs(ui)

    LA = 2

    def emit_scores(j):
        if jobs[j][3] is not None:
            jobs[j][3]()
        return scores(*jobs[j][0])

    q = [emit_scores(i) for i in range(min(LA, len(jobs)))]
    for i, (sa, pa, hook, pre) in enumerate(jobs):
        ps, pt = q.pop(0)
        expo(ps, pt, pa[0])
        while deferred and deferred[0][0] <= i:
            deferred.pop(0)[1]()
        if i + LA < len(jobs):
            q.append(emit_scores(i + LA))
        pv(pt, *pa)
        if hook is not None:
            hook(i)
    while deferred:
        deferred.pop(0)[1]()
    fw.release(m)


def mlstm_and_out(fw, cx, src, dst, W, C, l, b, gpre, gpost, ibfb, mng, cw, cb, yT_nsa, dbg, uT):
    w_in = W["mix_w_in"]
    m3 = fw.mark()
    mqT = fw.sb([128, 4, S], BF16, "mqT")
    mkT = fw.sb([128, 4, S], BF16, "mkT")
    mv = fw.sb([128, 16, 4, 129], BF16, "mv")
    sigo = fw.sb([128, 16, 512], BF16, "sigo")
    mif = fw.sb([128, 16, 8], F32, "mif")
    m3b = fw.mark()
    ws = WStream(fw, w_in, l)
    sgt = [fw.sb([128, 512], F32, "sgt") for _ in range(2)]
    raw = [fw.sb([128, 515], F32, "raw") for _ in range(4)]
    acc = [fw.sb([128, 512], F32, "acc") for _ in range(3)]
    fw.op("pool", lambda e: e.memset(mv[:, :, :, 128:129], 1.0), [], [mv])
    wbs = {C_MQ: ws.load(C_MQ, C_MQ + 512), C_MK: ws.load(C_MK, C_MK + 512)}
    pq = [fw.bank(0), fw.bank(1), fw.bank(2), fw.bank(3)]
    ptm = [fw.bank(4), fw.bank(5), fw.bank(6), fw.bank(7)]
    k = 0
    ri = 0
    for (coff, dstT, cbase) in ((C_MQ, mqT, 0), (C_MK, mkT, 4)):
        wb = wbs[coff]
        for h in range(4):
            c = cbase + h
            prev = None
            for tile in range(4):
                r = raw[ri % 4]
                ri += 1
                if prev is None:
                    fw.op("pool", lambda e, r=r: e.memset(r[:, 0:3], 0.0), [], [r])
                else:
                    fw.op("pool", lambda e, r=r, prev=prev: e.tensor_copy(out=r[:, 0:3], in_=prev[:, 512:515]), [prev], [r])
                p = pq[k % 4]
                a = acc[k % 3]
                k += 1
                proj_fm(fw, wb, h * 128, 128, uT, tile, p)
                fw.op("act", lambda e, p=p, r=r: e.activation(out=r[:, 3:515], in_=p[:, :], func=AF.Copy), [p], [r])
                fw.op("dve", lambda e, r=r, a=a: e.tensor_scalar(out=a[:], in0=r[:, 3:515], scalar1=cw[:, c, 3:4],
                                                                  scalar2=cb[:, c:c + 1], op0=ALU.mult, op1=ALU.add),
                      [r, cw, cb], [a])
                for j in (2, 1, 0):
                    fw.op("dve", lambda e, r=r, a=a, j=j: e.scalar_tensor_tensor(out=a[:], in0=r[:, j:j + 512],
                                                                                  scalar=cw[:, c, j:j + 1], in1=a[:],
                                                                                  op0=ALU.mult, op1=ALU.add),
                          [r, cw, a], [a])
                fw.op("act", lambda e, a=a, tile=tile: e.activation(out=dstT[:, h, tile * 512:(tile + 1) * 512], in_=a[:],
                                                                    func=AF.Silu), [a], [dstT])
                prev = r
    wb = ws.load(C_MV, C_MV + 512)
    wb_mo = ws.load(C_MO, C_MO + 512)
    for s in range(16):
        p = ptm[s % 4]
        proj_tm(fw, wb, 0, 512, uT, s, p)
        fw.op("act", lambda e, p=p, s=s: e.activation(out=mv[:, s, :, 0:128], in_=p[:, :].rearrange("p (h d) -> p h d", h=4),
                                                      func=AF.Copy), [p], [mv])
    wb = wb_mo
    for s in range(16):
        p = ptm[s % 4]
        proj_tm(fw, wb, 0, 512, uT, s, p)
        fw.op("act", lambda e, p=p, s=s: e.activation(out=sgt[s % 2][:], in_=p[:, :], func=AF.Sigmoid), [p], [sgt[s % 2]])
        fw.op("dve", lambda e, s=s: e.tensor_tensor(out=sigo[:, s, :], in0=sgt[s % 2][:], in1=mng[:], op=ALU.mult),
              [sgt[s % 2], mng], [sigo])
    wb = ws.load(C_MI, C_MI + 8)
    for s in range(16):
        p = ptm[s % 4]
        proj_tm(fw, wb, 0, 8, uT, s, p)
        fw.op("dve", lambda e, p=p, s=s: e.tensor_copy(out=mif[:, s, :], in_=p[:, 0:8]), [p], [mif])
    fw.release(m3b)
    wout = fw.sb([128, 8, D], BF16, "wout")
    fw.dma("pool", wout[:], W["mix_w_out"][l].rearrange("(kc p) n -> p kc n", p=128), reads=[W["mix_w_out"]], writes=[wout])
    tri = fw.sb([128, 128], BF16, "tri")
    trif = fw.sb([128, 128], F32, "trif")
    onesf = fw.sb([128, 128], F32, "onesf")
    fw.dma("pool", tri[:], C["c_tri"][:, :], reads=[C["c_tri"]], writes=[tri])
    fw.dma("sp", trif[:], C["c_tri"][:, :], reads=[C["c_tri"]], writes=[trif])
    fw.op("pool", lambda e: e.memset(onesf[:], 1.0), [], [onesf])
    Cf = fw.sb([128, 4, 129], F32, "Cf")
    Cb = [fw.sb([128, 4, 129], BF16, "Cb") for _ in range(2)]
    fw.op("pool", lambda e: e.memset(Cf[:], 0.0), [], [Cf])
    def G(name):
        return fw.sb([128, 16, 4], F32, name)
    z, e1, sp, av, a2, es, wk, ebt, EB = (G(n) for n in ("z", "e1", "sp", "av", "a2", "es", "wk", "ebt", "EB"))
    dn = fw.sb([128, 4], F32, "dn")
    coef = fw.sb([128, 4], F32, "coef")
    ssq4 = fw.sb([128, 4], F32, "ssq4")
    rstd4 = fw.sb([128, 4], F32, "rstd4")
    fs = fw.sb([128, 4], F32, "fs")
    PT = [fw.sb([128, 128], BF16, "PT") for _ in range(4)]
    k2 = [fw.sb([128, 128], BF16, "k2") for _ in range(4)]
    junk2 = fw.sb([128, 128], BF16, "junk2")
    junk = fw.sb([128, 512], BF16, "junk")
    ymb = fw.sb([128, 512], BF16, "ymb")
    yTm = fw.sb([128, 4, 128], BF16, "yTm")
    xres = [fw.sb([128, D], F32, "xres") for _ in range(2)]
    tmp = fw.sb([128, 512], F32, "tmp")
    tmpb = fw.sb([128, 512], F32, "tmpb")
    ssq2 = fw.sb([128, 2], F32, "ssq2")
    rs2 = fw.sb([128, 1], F32, "rs2")
    psT = [fw.bank(0, F32, h * 128, (h + 1) * 128) for h in range(4)]
    pk = [fw.bank(1, BF16, h * 64, (h + 1) * 64) for h in range(4)]
    pyt = fw.bank(1, BF16, 256, 512)
    pdC = [fw.bank(2, F32, 0, 129), fw.bank(2, F32, 129, 258), fw.bank(3, F32, 0, 129), fw.bank(3, F32, 129, 258)]
    pnum = [fw.bank(4, F32, 0, 129), fw.bank(4, F32, 129, 258), fw.bank(5, F32, 0, 129), fw.bank(5, F32, 129, 258)]
    ph = [fw.bank(6), fw.bank(7)]
    LN_MS = math.log(MSCALE)
    pcsA = fw.bank(2, F32, 0, 128)
    cumv = pcsA[:, 0:64].rearrange("p (c h) -> p c h", h=4)
    totv = pcsA[:, 64:128].rearrange("p (c h) -> p c h", h=4)
    fw.op("dve", lambda e: e.tensor_tensor(out=z[:], in0=mif[:, :, 4:8], in1=ibfb[:, 4:8].unsqueeze(1).broadcast_to([128, 16, 4]),
                                           op=ALU.add), [mif, ibfb], [z])
    fw.op("act", lambda e: e.activation(out=e1[:], in_=z[:], func=AF.Exp, scale=-1.0), [z], [e1])
    fw.op("act", lambda e: e.activation(out=sp[:], in_=e1[:], func=AF.Ln, bias=1.0), [e1], [sp])
    fw.op("pe", lambda e: e.matmul(pcsA[:, 0:64], lhsT=trif[:], rhs=sp[:].rearrange("p c h -> p (c h)"), start=True, stop=True),
          [trif, sp], [pcsA], inc=False)
    fw.op("pe", lambda e: e.matmul(pcsA[:, 64:128], lhsT=onesf[:], rhs=sp[:].rearrange("p c h -> p (c h)"), start=True, stop=True),
          [onesf, sp], [pcsA])
    fw.op("dve", lambda e: e.tensor_tensor(out=av[:], in0=mif[:, :, 0:4], in1=ibfb[:, 0:4].unsqueeze(1).broadcast_to([128, 16, 4]),
                                           op=ALU.add), [mif, ibfb], [av])
    fw.op("dve", lambda e: e.tensor_tensor(out=av[:], in0=av[:], in1=cumv, op=ALU.add), [av, pcsA], [av])
    fw.op("act", lambda e: e.activation(out=es[:], in_=av[:], func=AF.Exp), [av], [es])
    fw.op("dve", lambda e: e.tensor_tensor(out=a2[:], in0=av[:], in1=totv, op=ALU.subtract), [av, pcsA], [a2])
    fw.op("act", lambda e: e.activation(out=wk[:], in_=a2[:], func=AF.Exp), [a2], [wk])
    fw.op("act", lambda e: e.activation(out=ebt[:], in_=cumv, func=AF.Exp, scale=-1.0, bias=LN_MS), [pcsA], [ebt])
    fw.op("act", lambda e: e.activation(out=EB[:], in_=totv, func=AF.Exp, scale=-1.0), [pcsA], [EB])

    def A1(c):
        cc = slice(c * 128, (c + 1) * 128)
        i = c % 2
        r0 = b * S + c * 128
        fw.dma("sp", xres[i][:], src[r0:r0 + 128, :], reads=[src], writes=[xres[i]])
        for h in range(4):
            fw.op("pe", lambda e, h=h: e.matmul(psT[h][:, :], lhsT=mkT[:, h, cc], rhs=mqT[:, h, cc], start=True, stop=True),
                  [mkT, mqT], [psT[h]])
            fw.op("pe", lambda e, h=h: e.transpose(out=pk[h][:, :], in_=mkT[:, h, cc], identity=cx.ident[:]),
                  [mkT, cx.ident], [pk[h]])
        for h in range(4):
            fw.op("dve", lambda e, h=h: e.scalar_tensor_tensor(out=PT[h][:], in0=psT[h][:, :], scalar=es[:, c, h:h + 1],
                                                               in1=tri[:], op0=ALU.mult, op1=ALU.mult),
                  [psT[h], es, tri], [PT[h]])
            fw.op("dve", lambda e, h=h: e.tensor_scalar(out=k2[h][:], in0=pk[h][:, :], scalar1=wk[:, c, h:h + 1],
                                                        scalar2=None, op0=ALU.mult), [pk[h], wk], [k2[h]])

    def A2(c):
        cc = slice(c * 128, (c + 1) * 128)
        for h in range(4):
            fw.op("pe", lambda e, h=h: e.matmul(pdC[h][:, :], lhsT=k2[h][:], rhs=mv[:, c, h, :], start=True, stop=True),
                  [k2[h], mv], [pdC[h]])
            fw.op("pe", lambda e, h=h: e.matmul(pnum[h][:, :], lhsT=PT[h][:], rhs=mv[:, c, h, :], start=True, stop=(c == 0)),
                  [PT[h], mv], [pnum[h]], inc=(c == 0))
            if c > 0:
                fw.op("pe", lambda e, h=h: e.matmul(pnum[h][:, :], lhsT=mqT[:, h, cc], rhs=Cb[c % 2][:, h, :],
                                                    start=False, stop=True), [mqT, Cb[c % 2]], [pnum[h]])
        if c < 15:
            for h in range(4):
                fw.op("dve", lambda e, h=h: e.scalar_tensor_tensor(out=Cf[:, h, :], in0=Cf[:, h, :], scalar=EB[:, c, h:h + 1],
                                                                   in1=pdC[h][:, :], op0=ALU.mult, op1=ALU.add),
                      [Cf, EB, pdC[h]], [Cf])
            fw.op("act", lambda e: e.activation(out=Cb[(c + 1) % 2][:], in_=Cf[:], func=AF.Copy), [Cf], [Cb[(c + 1) % 2]])

    def B1(c):
        pn2 = [fw.bank(4), fw.bank(5)]
        for j in range(2):
            fw.op("dve", lambda e, j=j: e.tensor_tensor(out=dn[:, 2 * j:2 * j + 2], in0=pn2[j][:, 128:258:129],
                                                        in1=ebt[:, c, 2 * j:2 * j + 2], op=ALU.mult), [pn2[j], ebt], [dn])
        fw.op("dve", lambda e: e.tensor_scalar(out=coef[:], in0=dn[:], scalar1=-1.0, scalar2=1.0, op0=ALU.mult, op1=ALU.max),
              [dn], [coef])
        fw.op("dve", lambda e: e.tensor_tensor(out=dn[:], in0=dn[:], in1=coef[:], op=ALU.max), [dn, coef], [dn])
        fw.op("dve", lambda e: e.reciprocal(out=dn[:], in_=dn[:]), [dn], [dn])
        fw.op("dve", lambda e: e.tensor_tensor(out=coef[:], in0=dn[:], in1=ebt[:, c, :], op=ALU.mult), [dn, ebt], [coef])
        for h in range(4):
            fw.op("act", lambda e, h=h: e.activation(out=junk2[:], in_=pnum[h][:, 0:128], func=AF.Square,
                                                     accum_out=ssq4[:, h:h + 1]), [pnum[h]], [junk2, ssq4])
        fw.op("dve", lambda e: e.tensor_tensor(out=rstd4[:], in0=coef[:], in1=coef[:], op=ALU.mult), [coef], [rstd4])
        fw.op("dve", lambda e: e.tensor_tensor(out=rstd4[:], in0=rstd4[:], in1=ssq4[:], op=ALU.mult), [rstd4, ssq4], [rstd4])
        fw.op("act", lambda e: e.activation(out=rstd4[:], in_=rstd4[:], func=AF.Ln, scale=1.0 / 128, bias=EPS), [rstd4], [rstd4])
        fw.op("act", lambda e: e.activation(out=rstd4[:], in_=rstd4[:], func=AF.Exp, scale=-0.5), [rstd4], [rstd4])
        fw.op("dve", lambda e: e.tensor_tensor(out=fs[:], in0=coef[:], in1=rstd4[:], op=ALU.mult), [coef, rstd4], [fs])
        for h in range(4):
            hs = slice(h * 128, (h + 1) * 128)
            fw.op("dve", lambda e, h=h, hs=hs: e.scalar_tensor_tensor(out=ymb[:, hs], in0=pnum[h][:, 0:128], scalar=fs[:, h:h + 1],
                                                                      in1=sigo[:, c, hs], op0=ALU.mult, op1=ALU.mult),
                  [pnum[h], fs, sigo], [ymb])
        if dbg is not None and b == 0:
            fw.dma("pool", dbg["y_m"][c * 128:(c + 1) * 128, :], ymb[:], reads=[ymb], writes=[dbg["y_m"]])

    def B2(c):
        cc = slice(c * 128, (c + 1) * 128)
        i = c % 2
        r0 = b * S + c * 128
        transpose_to(fw, cx, ymb, yTm, slice(0, 128), pyt, nblk=4)
        for hf in range(2):
            for kc in range(8):
                if kc < 4:
                    lt, lb = yT_nsa[:, kc, cc], yT_nsa
                else:
                    lt, lb = yTm[:, kc - 4, :], yTm
                fw.op("pe", lambda e, kc=kc, hf=hf, lt=lt: e.matmul(ph[hf][:], lhsT=lt, rhs=wout[:, kc, hf * 512:(hf + 1) * 512],
                                                                    start=(kc == 0), stop=(kc == 7)),
                      [lb, wout], [ph[hf]], inc=(kc == 7))
        post_residual(fw, cx, ph[0], ph[1], xres[i], gpost, 1.0, ssq2, rs2, junk, tmp, lnexp=True, tmp2=tmpb)
        fw.dma("sp", dst[r0:r0 + 128, :], xres[i][:], reads=[xres[i]], writes=[dst])

    A1(0)
    A2(0)
    for c in range(16):
        if c + 1 < 16:
            A1(c + 1)
        B1(c)
        if c + 1 < 16:
            A2(c + 1)
        B2(c)
    fw.release(m3)


W_NAMES = ["norm_g", "ffn1_w_gu", "ffn1_w_down", "ffn2_w_gu", "ffn2_w_down", "mix_w_in", "mix_w_out",
           "nsa_cmp_pe", "nsa_cmp_w1", "nsa_cmp_w2", "mlstm_conv_w", "mlstm_conv_b", "mlstm_i_bias",
           "mlstm_f_bias", "mlstm_norm_g"]
W_SHAPES = {
    "norm_g": [2, 6, 1024], "ffn1_w_gu": [2, 1024, 5632], "ffn1_w_down": [2, 2816, 1024],
    "ffn2_w_gu": [2, 1024, 5632], "ffn2_w_down": [2, 2816, 1024], "mix_w_in": [2, 1024, 3360],
    "mix_w_out": [2, 1024, 1024], "nsa_cmp_pe": [2, 2, 32, 64], "nsa_cmp_w1": [2, 2, 2048, 128],
    "nsa_cmp_w2": [2, 2, 128, 64], "mlstm_conv_w": [2, 4, 1024], "mlstm_conv_b": [2, 1024],
    "mlstm_i_bias": [2, 4], "mlstm_f_bias": [2, 4], "mlstm_norm_g": [2, 512],
}


def host_consts():
    c = {}
    c["c_ident"] = np.eye(128, dtype=np.float32)
    pos = np.arange(S, dtype=np.float32)
    inv = (500000.0 ** (-np.arange(0, 16, 2, dtype=np.float32) / 16.0)).astype(np.float32)
    ang = (pos[None, :] * inv[:, None]).astype(np.float32)
    cs, sn = np.cos(ang).astype(np.float32), np.sin(ang).astype(np.float32)
    c["c_rope_c"] = np.concatenate([cs, cs], 0)
    c["c_rope_s"] = np.concatenate([-sn, sn], 0)
    perm = np.zeros((64, 16), np.float32)
    for i in range(8):
        perm[i + 8, i] = 1.0
        perm[i, i + 8] = 1.0
    c["c_perm"] = perm
    key = np.arange(S)
    c["c_E"] = (key[None, :] // 64 == np.arange(32)[:, None]).astype(np.float32)
    kk, qq = np.arange(128)[:, None], np.arange(128)[None, :]
    c["c_causal"] = np.where(kk > qq, -BIG, 0.0).astype(np.float32)
    c["c_anti"] = np.where(kk <= qq, -BIG, 0.0).astype(np.float32)
    n = np.arange(128)[:, None]
    c["c_cmpneg"] = np.where(16 * n + 31 > key[None, :], -BIG, 0.0).astype(np.float32)
    ovl = np.zeros((128, 33), np.float32)
    ovl[:, 0] = 1.0
    ci = np.arange(127)[:, None] * 16
    sj = np.arange(32)[None, :] * 64
    ovl[:127, 1:] = ((ci < sj + 64) & (ci + 32 > sj)).astype(np.float32)
    c["c_ovl"] = ovl
    t = np.arange(S)[:, None]
    j = np.arange(32)[None, :]
    cur = t // 64
    forced = (j == 0) | (j == cur) | (j == cur - 1)
    invalid = j * 64 > t
    c["c_addm"] = np.where(forced | invalid, -1e30, 0.0).astype(np.float32)
    c["c_forced"] = forced.astype(np.float32)
    c["c_tri"] = (kk <= qq).astype(np.float32)
    return c


def build(phases=("f1", "mix", "f2"), depth=DEPTH, ntiles=T // 512, debug=False):
    nc = bass.Bass("TRN2", target_bir_lowering=False)
    fw = FW(nc)
    cx = Ctx()
    x_in = fw.dram("x", [T, D], F32, kind="ExternalInput")
    y_out = fw.dram("y", [T, D], F32, kind="ExternalOutput")
    W = {n: fw.dram(n, W_SHAPES[n], F32, kind="ExternalInput") for n in W_NAMES}
    C = {n: fw.dram(n, list(v.shape), F32, kind="ExternalInput") for n, v in host_consts().items()}
    scr = [fw.dram("scrA", [T, D], F32), fw.dram("scrB", [T, D], F32)]

    cx.ident = fw.sb([128, 128], BF16, "ident")
    fw.dma("pool", cx.ident[:], C["c_ident"][:, :], reads=[C["c_ident"]], writes=[cx.ident])
    cx.perm = fw.sb([64, 16], BF16, "perm")
    fw.dma("pool", cx.perm[:], C["c_perm"][:, :], reads=[C["c_perm"]], writes=[cx.perm])
    dbg = None
    if debug:
        dbg = {"yT_nsa": fw.dram("dbg_yT_nsa", [128, 4, S], F32, kind="ExternalOutput"),
               "y_m": fw.dram("dbg_y_m", [S, 512], F32, kind="ExternalOutput")}
    cx.eps_t = fw.sb([128, 1], F32, "eps")
    cx.eps4_t = fw.sb([128, 1], F32, "eps4")
    fw.op("dve", lambda e: e.memset(cx.eps_t[:], EPS), [], [cx.eps_t])
    fw.op("dve", lambda e: e.memset(cx.eps4_t[:], 4.0 * EPS), [], [cx.eps4_t])

    plan = []
    for l in range(depth):
        for p in phases:
            plan.append((l, p))
    cur = x_in
    for i, (l, p) in enumerate(plan):
        dst = y_out if i == len(plan) - 1 else scr[i % 2]
        if p == "f1":
            ffn_phase(fw, cx, cur, dst, W["ffn1_w_gu"], W["ffn1_w_down"], W["norm_g"], l, 0, 1, ntiles)
        elif p == "f2":
            ffn_phase(fw, cx, cur, dst, W["ffn2_w_gu"], W["ffn2_w_down"], W["norm_g"], l, 4, 5, ntiles)
        else:
            mixer_phase(fw, cx, cur, dst, W, C, l, dbg)
        cur = dst
    fw.finish()
    return nc


_CACHE = {}


def kernel(**inputs):
    x = np.ascontiguousarray(np.asarray(inputs["x"], dtype=np.float32))
    ncores = 8
    if "nc" not in _CACHE:
        _CACHE["nc"] = build()
    nc = _CACHE["nc"]
    consts = host_consts()
    shared = {n: np.ascontiguousarray(np.asarray(inputs[n], dtype=np.float32)) for n in W_NAMES}
    shared.update(consts)
    in_maps = []
    for c in range(ncores):
        m = dict(shared)
        m["x"] = x[c * NB:(c + 1) * NB].reshape(T, D)
        in_maps.append(m)
    res = run_bass_kernel_spmd(nc, in_maps, core_ids=list(range(ncores)))
    out = np.concatenate([r["y"].reshape(NB, S, D) for r in res.results], axis=0)
    return out.astype(np.float32)
```
